# Optimizing a Trainium2 kernel written in Bass

```python
import jax, jax.numpy as jnp
from jax import lax
import numpy as np

D_MODEL = 1024
BATCH = 8
SEQ = 4096
DEPTH = 1

CHUNK = 64
D_MIX = D_MODEL
SB_WIDTH = D_MIX // 2
SB_HEADS = 8
SB_HEAD_DIM = SB_WIDTH // SB_HEADS
SB_BLOCK = 128
HG_WIDTH = D_MIX - SB_WIDTH
HG_EXPAND = 128
HG_HEADS = HG_WIDTH // HG_EXPAND
PLE_DIM = 256
N_GROUPS = 4
EXPERTS_PER_GROUP = 8
N_EXPERTS = N_GROUPS * EXPERTS_PER_GROUP
TOP_K = 2
D_EXPERT = D_MODEL // 2
EPS = 1e-6
IN_COLS = 3 * SB_WIDTH + 4 * HG_WIDTH

kernel_name = "hymba_sb_hgrn2_hmoe_ple"


def rmsnorm(x, w):
    xf = x.astype(jnp.float32)
    y = xf * lax.rsqrt(jnp.mean(xf * xf, axis=-1, keepdims=True) + EPS)
    return (y * w.astype(jnp.float32)).astype(x.dtype)


def head_rmsnorm(x, w):
    h, dh = x.shape[-2], x.shape[-1]
    y = x * lax.rsqrt(jnp.mean(x * x, axis=-1, keepdims=True) + EPS)
    return y * w.astype(jnp.float32).reshape(h, dh)


def stick_breaking_attention(q, k, v):
    s_len, dh = q.shape[2], q.shape[3]
    scale = dh ** -0.5
    outs = []
    for blk in range(s_len // SB_BLOCK):
        q0 = blk * SB_BLOCK
        q1 = q0 + SB_BLOCK
        qb = q[:, :, q0:q1]
        kb = k[:, :, :q1]
        vb = v[:, :, :q1]
        z = jnp.einsum('bhtd,bhsd->bhts', qb, kb) * scale
        t_pos = q0 + jnp.arange(SB_BLOCK)[:, None]
        s_pos = jnp.arange(q1)[None, :]
        before = s_pos < t_pos
        log_keep = jnp.where(before, jax.nn.log_sigmoid(-z), 0.0)
        between = lax.cumsum(log_keep, axis=3, reverse=True) - log_keep
        log_a = jax.nn.log_sigmoid(z) + between
        a = jnp.where(before, jnp.exp(log_a), 0.0)
        outs.append(jnp.einsum('bhts,bhsd->bhtd', a, vb))
    return jnp.concatenate(outs, axis=2)


def hgrn2_chunkwise(q, k, v, log_f):
    b, h, s_len, dk = q.shape
    dv = v.shape[-1]
    n = s_len // CHUNK

    def to_chunks(t):
        return jnp.moveaxis(t.reshape(b, h, n, CHUNK, t.shape[-1]), 2, 0)

    causal = jnp.tril(jnp.ones((CHUNK, CHUNK), dtype=bool))

    def step(state, inp):
        qc, kc, vc, gc = inp
        bc = jnp.cumsum(gc, axis=2)
        diff = bc[:, :, :, None, :] - bc[:, :, None, :, :]
        decay = jnp.exp(jnp.where(causal[:, :, None], diff, -jnp.inf))
        scores = jnp.einsum('bhtk,bhsk,bhtsk->bhts', qc, kc, decay)
        o = (jnp.einsum('bhts,bhsv->bhtv', scores, vc)
             + jnp.einsum('bhtk,bhkv->bhtv', qc * jnp.exp(bc), state))
        b_last = bc[:, :, -1:, :]
        new_state = (jnp.exp(b_last[:, :, 0, :, None]) * state
                     + jnp.einsum('bhsk,bhsv->bhkv', kc * jnp.exp(b_last - bc), vc))
        return new_state, o

    s0 = jnp.zeros((b, h, dk, dv), q.dtype)
    _, o = lax.scan(step, s0, (to_chunks(q), to_chunks(k), to_chunks(v), to_chunks(log_f)))
    return jnp.moveaxis(o, 0, 2).reshape(b, h, s_len, dv)


def hierarchical_moe(x, w_group_router, b_group_router, w_expert_router, b_expert_router,
                     w_gate, w_up, w_down):
    bsz, s_len, d = x.shape
    xt = x.reshape(-1, d)
    n = xt.shape[0]
    xf = xt.astype(jnp.float32)
    group_logits = xf @ w_group_router.astype(jnp.float32) + b_group_router.astype(jnp.float32)
    group_probs = jax.nn.softmax(group_logits, axis=-1)
    g_idx = jnp.argmax(group_logits, axis=-1).astype(jnp.int32)
    g_prob = jnp.take_along_axis(group_probs, g_idx[:, None], axis=1)
    expert_logits = (xf @ w_expert_router.astype(jnp.float32)
                     + b_expert_router.astype(jnp.float32)).reshape(n, N_GROUPS, EXPERTS_PER_GROUP)
    in_group = jnp.take_along_axis(expert_logits, g_idx[:, None, None], axis=1)[:, 0]
    top_vals, top_idx = lax.top_k(in_group, TOP_K)
    gates = jax.nn.softmax(top_vals, axis=-1) * g_prob
    expert_ids = (g_idx[:, None] * EXPERTS_PER_GROUP + top_idx.astype(jnp.int32)).reshape(-1)
    order = jnp.argsort(expert_ids)
    token_ids = order // TOP_K
    group_sizes = jnp.bincount(expert_ids, length=N_EXPERTS).astype(jnp.int32)
    xs = xt[token_ids]
    hg = lax.ragged_dot(xs, w_gate.astype(xs.dtype), group_sizes)
    hu = lax.ragged_dot(xs, w_up.astype(xs.dtype), group_sizes)
    y = lax.ragged_dot(jax.nn.silu(hg) * hu, w_down.astype(xs.dtype), group_sizes)
    y = y * gates.reshape(-1)[order][:, None].astype(y.dtype)
    out = jnp.zeros_like(xt).at[token_ids].add(y.astype(xt.dtype))
    return out.reshape(bsz, s_len, d)


def setup_inputs(seed: int = 0) -> dict:
    key = jax.random.key(seed)
    ks = jax.random.split(key, 20)
    f32 = jnp.float32

    def nrm(k, shape, scale):
        return jax.random.normal(k, shape, f32) * scale

    def gain(k, shape):
        return 1.0 + 0.01 * jax.random.normal(k, shape, f32)

    return {
        "x": nrm(ks[0], (BATCH, SEQ, D_MODEL), 1.0),
        "p": nrm(ks[1], (DEPTH, BATCH, SEQ, PLE_DIM), 1.0),
        "attn_norm_w": gain(ks[2], (DEPTH, D_MODEL)),
        "w_in": nrm(ks[3], (DEPTH, D_MODEL, IN_COLS), D_MODEL ** -0.5),
        "sb_norm_w": gain(ks[4], (DEPTH, SB_WIDTH)),
        "hg_lower_bounds": nrm(ks[5], (DEPTH + 1, HG_WIDTH), 0.5),
        "hg_norm_w": gain(ks[6], (DEPTH, HG_WIDTH)),
        "w_out": nrm(ks[7], (DEPTH, D_MIX, D_MODEL), D_MIX ** -0.5),
        "ffn_norm_w": gain(ks[8], (DEPTH, D_MODEL)),
        "w_group_router": nrm(ks[9], (DEPTH, D_MODEL, N_GROUPS), D_MODEL ** -0.5),
        "b_group_router": nrm(ks[10], (DEPTH, N_GROUPS), 0.01),
        "w_expert_router": nrm(ks[11], (DEPTH, D_MODEL, N_EXPERTS), D_MODEL ** -0.5),
        "b_expert_router": nrm(ks[12], (DEPTH, N_EXPERTS), 0.01),
        "w_exp_gate": nrm(ks[13], (DEPTH, N_EXPERTS, D_MODEL, D_EXPERT), D_MODEL ** -0.5),
        "w_exp_up": nrm(ks[14], (DEPTH, N_EXPERTS, D_MODEL, D_EXPERT), D_MODEL ** -0.5),
        "w_exp_down": nrm(ks[15], (DEPTH, N_EXPERTS, D_EXPERT, D_MODEL), D_EXPERT ** -0.5),
        "ple_norm_w": gain(ks[16], (DEPTH, D_MODEL)),
        "w_ple_proj": nrm(ks[17], (DEPTH, PLE_DIM, D_MODEL), PLE_DIM ** -0.5),
        "w_ple_gate": nrm(ks[18], (DEPTH, D_MODEL, D_MODEL), D_MODEL ** -0.5),
        "final_norm_w": gain(ks[19], (D_MODEL,)),
    }


def reference(x, p, attn_norm_w, w_in, sb_norm_w, hg_lower_bounds, hg_norm_w, w_out,
              ffn_norm_w, w_group_router, b_group_router, w_expert_router, b_expert_router,
              w_exp_gate, w_exp_up, w_exp_down, ple_norm_w, w_ple_proj, w_ple_gate,
              final_norm_w):
    f32 = jnp.float32
    bsz, s_len, _ = x.shape
    lower_bounds = jnp.cumsum(jax.nn.softmax(hg_lower_bounds.astype(f32), axis=0), axis=0)
    h = x
    for i in range(DEPTH):
        a = rmsnorm(h, attn_norm_w[i])
        proj = (a @ w_in[i]).astype(f32)
        sb_q, sb_k, sb_v, hg_q, hg_f, hg_i, hg_g = jnp.split(
            proj, np.cumsum([SB_WIDTH] * 3 + [HG_WIDTH] * 3).tolist(), axis=-1)

        def sb_heads(t):
            return t.reshape(bsz, s_len, SB_HEADS, SB_HEAD_DIM).transpose(0, 2, 1, 3)

        sb_o = stick_breaking_attention(sb_heads(sb_q), sb_heads(sb_k), sb_heads(sb_v))
        sb_o = head_rmsnorm(sb_o.transpose(0, 2, 1, 3), sb_norm_w[i])
        sb_o = sb_o.reshape(bsz, s_len, SB_WIDTH)

        lb = lower_bounds[i]
        f = lb + (1.0 - lb) * jax.nn.sigmoid(hg_f)
        log_f = jnp.log(f)
        hk = 1.0 - f

        def hg_heads(t):
            return t.reshape(bsz, s_len, HG_HEADS, HG_EXPAND).transpose(0, 2, 1, 3)

        hg_o = hgrn2_chunkwise(hg_heads(jax.nn.silu(hg_q)), hg_heads(hk),
                               hg_heads(hg_i), hg_heads(log_f))
        hg_o = head_rmsnorm(hg_o.transpose(0, 2, 1, 3), hg_norm_w[i]).reshape(bsz, s_len, HG_WIDTH)
        hg_o = hg_o * jax.nn.silu(hg_g)

        mix = jnp.concatenate([sb_o, hg_o], axis=-1).astype(h.dtype)
        h = h + (mix @ w_out[i]).astype(h.dtype)

        m = rmsnorm(h, ffn_norm_w[i])
        h = h + hierarchical_moe(m, w_group_router[i], b_group_router[i], w_expert_router[i],
                                 b_expert_router[i], w_exp_gate[i], w_exp_up[i],
                                 w_exp_down[i]).astype(h.dtype)

        e = (p[i].astype(h.dtype) @ w_ple_proj[i]).astype(f32)
        gate = jax.nn.sigmoid((rmsnorm(h, ple_norm_w[i]) @ w_ple_gate[i]).astype(f32))
        h = h + (gate * e).astype(h.dtype)
    return rmsnorm(h, final_norm_w)
```

```python
import numpy as np
import concourse.bass as bass
import concourse.mybir as mybir
from concourse.bass_utils import run_bass_kernel_spmd
from contextlib import ExitStack

F32 = mybir.dt.float32
BF16 = mybir.dt.bfloat16
I32 = mybir.dt.int32
AF = mybir.ActivationFunctionType
ALU = mybir.AluOpType
AX = mybir.AxisListType

COMPUTE = ("pe", "act", "dve", "pool")
ENGS = ("pe", "act", "dve", "pool", "sp")


class Buf:
    __slots__ = ("name", "lw", "rdc", "rdd")

    def __init__(self, name=""):
        self.name = name
        self.lw = None
        self.rdc = {}
        self.rdd = []


class Op:
    __slots__ = ("eng", "fn", "cdeps", "ddeps", "idx", "sig", "signo", "dma", "dsem", "dval", "gen")


class Prog:
    def __init__(self, n_dma_sems=20):
        self.ops = {e: [] for e in ENGS}
        self.n_dma_sems = n_dma_sems
        self.gen = 0
        self.barriers = []

    def barrier(self):
        self.barriers.append({e: len(self.ops[e]) for e in ENGS})
        self.gen += 1

    def add(self, eng, fn, reads=(), writes=(), dma=False):
        op = Op()
        op.eng = eng
        op.fn = fn
        op.dma = dma
        op.idx = len(self.ops[eng])
        op.cdeps = {}
        op.ddeps = []
        op.sig = False
        op.signo = 0
        op.dsem = None
        op.dval = 0
        op.gen = self.gen

        def dep(p):
            if p is None or p is op:
                return
            if p.dma:
                if p not in op.ddeps:
                    op.ddeps.append(p)
            else:
                if (not dma) and p.eng == "pe" and eng == "pe":
                    return
                cur = op.cdeps.get(p.eng)
                if cur is None or cur.idx < p.idx:
                    op.cdeps[p.eng] = p

        for b in reads:
            dep(b.lw)
        for b in writes:
            dep(b.lw)
            for r in b.rdc.values():
                dep(r)
            for r in b.rdd:
                dep(r)
        for b in reads:
            if dma:
                b.rdd.append(op)
            else:
                b.rdc[eng] = op
        for b in writes:
            b.lw = op
            b.rdc = {}
            b.rdd = []
        self.ops[eng].append(op)
        return op

    def pe(self, fn, reads=(), writes=()):
        return self.add("pe", fn, reads, writes)

    def act(self, fn, reads=(), writes=()):
        return self.add("act", fn, reads, writes)

    def dve(self, fn, reads=(), writes=()):
        return self.add("dve", fn, reads, writes)

    def pool(self, fn, reads=(), writes=()):
        return self.add("pool", fn, reads, writes)

    def dma(self, fn, reads=(), writes=(), q="sp"):
        return self.add(q, fn, reads, writes, dma=True)

    def emit(self, nc, stack):
        for e in ENGS:
            for op in self.ops[e]:
                for p in op.cdeps.values():
                    p.sig = True
        for snap in self.barriers:
            for e in COMPUTE:
                for op in reversed(self.ops[e][:snap[e]]):
                    if not op.dma:
                        op.sig = True
                        break
        for e in COMPUTE:
            n = 0
            for op in self.ops[e]:
                if op.sig and not op.dma:
                    n += 1
                    op.signo = n
        esem = {e: stack.enter_context(nc.semaphore("es_" + e)) for e in COMPUTE}
        dpool = {}
        for q in ("sp", "pool", "act"):
            if any(o.dma for o in self.ops[q]):
                dpool[q] = [stack.enter_context(nc.semaphore("ds_%s_%d" % (q, i)))
                            for i in range(self.n_dma_sems)]
        for q, sems in dpool.items():
            n = 0
            for op in self.ops[q]:
                if op.dma:
                    op.dsem = sems[n % len(sems)]
                    op.dval = 16 * (n // len(sems) + 1)
                    n += 1
        bar_waits = []
        for snap in self.barriers:
            wl = []
            for e in COMPUTE:
                for op in reversed(self.ops[e][:snap[e]]):
                    if not op.dma:
                        wl.append((esem[e], op.signo))
                        break
            for q in dpool:
                seen = {}
                for op in self.ops[q][:snap[q]]:
                    if op.dma:
                        seen[id(op.dsem)] = (op.dsem, op.dval)
                wl.extend(seen.values())
            bar_waits.append(wl)
        block = stack.enter_context(nc.Block())
        prog = self

        def run(ename, eng):
            waited = {}

            def wait(sem, val):
                k = id(sem)
                if waited.get(k, 0) >= val:
                    return
                eng.wait_ge(sem, val)
                waited[k] = val

            cur_gen = 0
            for op in prog.ops[ename]:
                while cur_gen < op.gen:
                    for sem, val in bar_waits[cur_gen]:
                        wait(sem, val)
                    cur_gen += 1
                for p in op.cdeps.values():
                    wait(esem[p.eng], p.signo)
                for p in op.ddeps:
                    wait(p.dsem, p.dval)
                if op.dma:
                    if op.dval > 16:
                        wait(op.dsem, op.dval - 16)
                    ins = op.fn(eng)
                    ins.then_inc(op.dsem, 16)
                else:
                    ins = op.fn(eng)
                    if op.sig:
                        ins.then_inc(esem[ename], 1)
            last = {}
            for op in prog.ops[ename]:
                if op.dma:
                    last[id(op.dsem)] = (op.dsem, op.dval)
            for sem, val in last.values():
                wait(sem, val)

        if self.ops["sp"]:
            @block.sync
            def _(e):
                run("sp", e)
        if self.ops["pe"]:
            @block.tensor
            def _(e):
                run("pe", e)
        if self.ops["act"]:
            @block.scalar
            def _(e):
                run("act", e)
        if self.ops["dve"]:
            @block.vector
            def _(e):
                run("dve", e)
        if self.ops["pool"]:
            @block.gpsimd
            def _(e):
                run("pool", e)

D = 1024
EPS = 1e-6
NEXP = 32
WB = 5
NCB = 128 * 5 + 4 * 512 + 8 + 1024 + 128
NCF = 256


class Arena:
    def __init__(self, nc, stack, n4):
        self.t = stack.enter_context(nc.sbuf_tensor("arena", [128, n4], F32))
        self.off = 0
        self.cap = n4

    def alloc(self, shape, dt):
        n = 1
        for s_ in shape[1:]:
            n *= s_
        nb = n * (2 if dt == BF16 else 4)
        n4 = (nb + 3) // 4
        n4 = (n4 + 7) // 8 * 8
        assert self.off + n4 <= self.cap, ("arena overflow", self.off, n4, self.cap)
        v = self.t[:, self.off:self.off + n4]
        self.off += n4
        if dt != F32:
            v = v.bitcast(dt)
        v = v[:, 0:n]
        if len(shape) == 3:
            v = v.rearrange("p (a b) -> p a b", a=shape[1])
        elif len(shape) == 4:
            v = v.rearrange("p (a b c) -> p a b c", a=shape[1], b=shape[2])
        return v


def run_interleaved(gens, W):
    pending = list(gens)
    active = [pending.pop(0) for _ in range(min(W, len(pending)))]
    while active:
        for g in list(active):
            try:
                next(g)
            except StopIteration:
                active.remove(g)
                if pending:
                    active.append(pending.pop(0))


class Rot:
    def __init__(self, items):
        self.items = list(items)
        self.i = 0

    def next(self):
        v = self.items[self.i % len(self.items)]
        self.i += 1
        return v


def build(S=4096, stop_after=None, debug_mix=False):
    nc = bass.Bass("TRN2", target_bir_lowering=False)
    NT = S // 128
    NS = S // 512
    PT = min(16, NT)
    NPASS = NT // PT

    def din(name, shape, dt=F32):
        return nc.dram_tensor(name, shape, dt, kind="ExternalInput").ap()

    x = din("x", [S, D])
    p_in = din("p", [S, 256])
    w_in = din("w_in", [D, 3584])
    w_out = din("w_out", [D, D])
    w_r = din("w_r", [D, 36])
    b_r = din("b_r", [36])
    w_eg = din("w_eg", [32, D, 512])
    w_eu = din("w_eu", [32, D, 512])
    w_ed = din("w_ed", [32, 512, D])
    w_pp = din("w_pp", [256, D])
    w_pg = din("w_pg", [D, D])
    anw = din("anw", [128, 8])
    sbw = din("sbw", [512])
    hlb = din("hlb", [2, 512])
    hgw = din("hgw", [512])
    fnw = din("fnw", [128, 8])
    plw = din("plw", [128, 8])
    finw = din("finw", [D])
    fnwv = din("fnwv", [D])
    cbd = din("cb", [128, NCB])
    cfd = din("cf", [128, NCF])
    out = nc.dram_tensor("out", [S, D], F32, kind="ExternalOutput").ap()
    pairid_d = nc.dram_tensor("pairid", [128, 2 * NT], I32, kind="ExternalInput").ap()
    linit_d = nc.dram_tensor("linit", [128, 2 * NT + 64], I32, kind="ExternalInput").ap()
    sstart_d = din("sstart", [128, 2 * NT + 64])
    piota_d = din("piota", [128, 1])
    m_scr = nc.dram_tensor("m_scr", [2 * S + 128, D], BF16, kind="Internal").ap()
    h1_scr = nc.dram_tensor("h1_scr", [S, D], F32, kind="Internal").ap()
    y_scr = nc.dram_tensor("y_scr", [2 * S + 128, D], F32, kind="Internal").ap()
    lst_i = nc.dram_tensor("lst_i", [(2 * NT + 64) * 128, 1], I32, kind="Internal").ap()
    lst_g = nc.dram_tensor("lst_g", [(2 * NT + 64) * 128, 1], F32, kind="Internal").ap()
    wgb = nc.dram_tensor("wgb", [32 * 128, 4096], BF16, kind="Internal").ap()
    wub = nc.dram_tensor("wub", [32 * 128, 4096], BF16, kind="Internal").ap()
    wdb = nc.dram_tensor("wdb", [32 * 128, 4096], BF16, kind="Internal").ap()
    if debug_mix:
        mix = nc.dram_tensor("mix_scr", [S, D], BF16, kind="ExternalOutput").ap()
    else:
        mix = nc.dram_tensor("mix_scr", [S, D], BF16, kind="Internal").ap()

    st = ExitStack()
    with st:
        P = Prog()
        AR = Arena(nc, st, 53000)
        pb = [st.enter_context(nc.psum_tensor("pb%d" % i, [128, 512], F32)) for i in range(8)]
        b_pb = [Buf("pb%d" % i) for i in range(8)]

        def mm(o, lhsT, rhs, start, stop, r, w):
            P.pe(lambda e, o=o, l=lhsT, rh=rhs, s0=start, s1=stop: e.matmul(o, lhsT=l, rhs=rh, start=s0, stop=s1), r, w)

        def tr(o, i, ident, r, w):
            P.pe(lambda e, o=o, i=i, d=ident: e.transpose(out=o, in_=i, identity=d), r, w)

        def actf(o, i, func, r, w, scale=1.0, bias=0.0, accum=None):
            if accum is None:
                P.act(lambda e, o=o, i=i, f=func, s=scale, b=bias: e.activation(out=o, in_=i, func=f, scale=s, bias=b), r, w)
            else:
                P.act(lambda e, o=o, i=i, f=func, s=scale, b=bias, a=accum: e.activation(out=o, in_=i, func=f, scale=s, bias=b, accum_out=a), r, w)

        def tt(eng, o, a, b, op, r, w):
            P.add(eng, lambda e, o=o, a=a, b=b, op=op: e.tensor_tensor(out=o, in0=a, in1=b, op=op), r, w)

        def ts(eng, o, a, s1, s2, op0, op1, r, w):
            if s2 is None:
                P.add(eng, lambda e, o=o, a=a, s1=s1, op0=op0: e.tensor_scalar(out=o, in0=a, scalar1=s1, scalar2=None, op0=op0), r, w)
            else:
                P.add(eng, lambda e, o=o, a=a, s1=s1, s2=s2, op0=op0, op1=op1: e.tensor_scalar(out=o, in0=a, scalar1=s1, scalar2=s2, op0=op0, op1=op1), r, w)

        def stt(eng, o, a, sc, b, op0, op1, r, w):
            P.add(eng, lambda e, o=o, a=a, sc=sc, b=b, op0=op0, op1=op1: e.scalar_tensor_tensor(out=o, in0=a, scalar=sc, in1=b, op0=op0, op1=op1), r, w)

        def cp(eng, o, i, r, w):
            if eng == "act":
                actf(o, i, AF.Copy, r, w)
            else:
                P.add(eng, lambda e, o=o, i=i: e.tensor_copy(out=o, in_=i), r, w)

        def recip(o, i, r, w):
            P.dve(lambda e, o=o, i=i: e.reciprocal(out=o, in_=i), r, w)

        def recip_act(o, i, r, w):
            actf(o, i, AF.Ln, r, w)
            actf(o, o, AF.Exp, w, w, scale=-1.0)

        def dma(o, i, r, w, q="sp"):
            P.dma(lambda e, o=o, i=i: e.dma_start(out=o, in_=i), r, w, q=q)

        def rstd_from_ss(ss_ap, n, r_buf):
            actf(ss_ap, ss_ap, AF.Ln, [r_buf], [r_buf], scale=1.0 / n, bias=EPS)
            actf(ss_ap, ss_ap, AF.Exp, [r_buf], [r_buf], scale=-0.5)

        def bf16view(bank):
            return bank[:, :].bitcast(BF16)

        cb = AR.alloc([128, NCB], BF16)
        b_cb = Buf("cb")
        cf = AR.alloc([128, NCF], F32)
        b_cf = Buf("cf")
        dma(cb, cbd, [], [b_cb], q="pool")
        dma(cf, cfd, [], [b_cb])
        idb = cb[:, 0:128]
        nuincl = cb[:, 128:256]
        nones = cb[:, 256:384]
        dmask = [cb[:, 640 + 512 * j: 640 + 512 * (j + 1)] for j in range(4)]
        pm0 = cb[:, 2688:2689]
        pm1 = cb[:, 2689:2690]
        cm0 = cb[:, 2696:2696 + 512]
        cm1 = cb[:, 2696 + 512:2696 + 1024]
        pones = cb[:, 3720:3848]
        trifwd = cb[:, 384:512]
        trirev = cb[:, 512:640]
        base_mark = AR.off

        aT = AR.alloc([128, 8, S], BF16)
        b_aT = [Buf("aT%d" % t) for t in range(NT)]
        nwA = AR.alloc([128, 8], F32)
        b_nwA = Buf()
        dma(nwA, anw, [], [b_nwA])
        sbw_b = AR.alloc([128, 512], F32)
        b_sbw = Buf()
        dma(sbw_b, sbw.partition_broadcast(128), [], [b_sbw])
        hgw_b = AR.alloc([128, 512], F32)
        b_hgw = Buf()
        dma(hgw_b, hgw.partition_broadcast(128), [], [b_hgw])
        lbraw = AR.alloc([128, 2, 512], F32)
        b_lbraw = Buf()
        dma(lbraw, hlb.partition_broadcast(128), [], [b_lbraw])
        oml_b = AR.alloc([128, 512], F32)
        b_oml = Buf()
        tt("dve", oml_b, lbraw[:, 1, :], lbraw[:, 0, :], ALU.subtract, [b_lbraw], [b_oml])
        actf(oml_b, oml_b, AF.Exp, [b_oml], [b_oml])
        ts("dve", lbraw[:, 0, :], oml_b, 1.0, None, ALU.add, None, [b_oml], [b_lbraw])
        recip(lbraw[:, 0, :], lbraw[:, 0, :], [b_lbraw], [b_lbraw])
        tt("dve", oml_b, oml_b, lbraw[:, 0, :], ALU.mult, [b_oml, b_lbraw], [b_oml])

        junk = AR.alloc([128, D], F32)
        b_junk = Buf()
        ssA = AR.alloc([128, NT], F32)
        b_ssA = [Buf() for _ in range(NT)]
        a1_mark = AR.off
        xs = [AR.alloc([128, D], F32) for _ in range(WB)]
        b_xs = [Buf() for _ in range(WB)]
        xn = [AR.alloc([128, D], BF16) for _ in range(WB)]
        b_xn = [Buf() for _ in range(WB)]
        psA = Rot([6, 7])
        def a1_tile(t):
            xt, bx = xs[t % WB], b_xs[t % WB]
            dma(xt, x[t * 128:(t + 1) * 128, :], [], [bx])
            sst = ssA[:, t:t + 1]
            actf(junk, xt, AF.Square, [bx], [b_junk, b_ssA[t]], accum=sst)
            rstd_from_ss(sst, D, b_ssA[t])
            yield
            xnt, bxn = xn[t % WB], b_xn[t % WB]
            ts("dve", xnt, xt, sst, None, ALU.mult, None, [bx, b_ssA[t]], [bxn])
            yield
            bi = psA.next()
            pv = bf16view(pb[bi]).rearrange("p (k n) -> p k n", k=8)
            for k in range(8):
                tr(pv[:, k, :], xnt[:, k * 128:(k + 1) * 128], idb, [bxn, b_cb], [b_pb[bi]])
            for k in range(8):
                ts("dve", aT[:, k, t * 128:(t + 1) * 128], pv[:, k, :], nwA[:, k:k + 1], None, ALU.mult, None,
                   [b_pb[bi], b_nwA], [b_aT[t]])

            yield
        run_interleaved([a1_tile(t) for t in range(NT)], WB)
        P.barrier()
        AR.off = a1_mark

        def aT_bufs(tt_):
            return [b_aT[4 * tt_ + j] for j in range(4)]

        qT = AR.alloc([128, S], BF16)
        b_qT = [Buf() for _ in range(NS)]
        kT = AR.alloc([128, S], BF16)
        b_kT = [Buf() for _ in range(NS)]
        Vp = AR.alloc([128, NT, 128], BF16)
        b_Vp = [Buf() for _ in range(NS)]
        wsl = [AR.alloc([128, 8, 128], BF16) for _ in range(8)]
        b_wsl = [Buf() for _ in range(8)]
        wrot = Rot(range(8))

        def load_w(col0):
            i = wrot.next()
            dma(wsl[i], w_in[:, col0:col0 + 128].rearrange("(k p) n -> p k n", p=128), [], [b_wsl[i]], q="pool")
            return i

        def proj_fm(wi, dst, dst_bufs, scale, evac_rot, prot):
            for g in range(NS):
                bi = prot.next()
                for k in range(8):
                    mm(pb[bi][:, :], wsl[wi][:, k, :], aT[:, k, g * 512:(g + 1) * 512], k == 0, k == 7,
                       aT_bufs(g) + [b_wsl[wi]], [b_pb[bi]])
                if evac_rot.next() == 0:
                    actf(dst[:, g * 512:(g + 1) * 512], pb[bi][:, :], AF.Copy, [b_pb[bi]], [dst_bufs[g]], scale=scale)
                else:
                    ts("dve", dst[:, g * 512:(g + 1) * 512], pb[bi][:, :], scale, None, ALU.mult, None, [b_pb[bi]], [dst_bufs[g]])

        def proj_tm_group(wi, g, bi):
            pvw = pb[bi][:, :].rearrange("p (j n) -> p j n", j=4)
            for j in range(4):
                t = 4 * g + j
                for k in range(8):
                    mm(pvw[:, j, :], aT[:, k, t * 128:(t + 1) * 128], wsl[wi][:, k, :], k == 0, k == 7,
                       [b_aT[t], b_wsl[wi]], [b_pb[bi]])
            return pvw

        sb_mark = AR.off
        NB = 3
        Eb = [AR.alloc([128, 512], F32) for _ in range(NB)]
        b_E = [Buf() for _ in range(NB)]
        Lpb = [AR.alloc([128, 512], BF16) for _ in range(NB)]
        b_Lp = [Buf() for _ in range(NB)]
        ATb = [AR.alloc([128, 512], BF16) for _ in range(NB)]
        b_AT = [Buf() for _ in range(NB)]
        Saccs = [AR.alloc([128, 512], BF16) for _ in range(2)]
        b_Sacc = [Buf() for _ in range(2)]
        qT_b = AR.alloc([128, S], BF16)
        kT_b = AR.alloc([128, S], BF16)
        Vp_b = AR.alloc([128, NT, 128], BF16)
        QTs, KTs, VPs = [qT, qT_b], [kT, kT_b], [Vp, Vp_b]
        B_QT = [b_qT, [Buf() for _ in range(NS)]]
        B_KT = [b_kT, [Buf() for _ in range(NS)]]
        B_VP = [b_Vp, [Buf() for _ in range(NS)]]

        def proj_pair_gen(pr_):
            c_ = pr_ % 2
            wq_i = load_w(pr_ * 128)
            wk_i = load_w(512 + pr_ * 128)
            wv_i = load_w(1024 + pr_ * 128)
            yield
            for wi_, dst_, bl_, sc_ in ((wq_i, QTs[c_], B_QT[c_], 0.125), (wk_i, KTs[c_], B_KT[c_], 1.0)):
                for g in range(NS):
                    for k in range(8):
                        mm(pb[7][:, :], wsl[wi_][:, k, :], aT[:, k, g * 512:(g + 1) * 512], k == 0, k == 7,
                           aT_bufs(g) + [b_wsl[wi_]], [b_pb[7]])
                    ts("dve", dst_[:, g * 512:(g + 1) * 512], pb[7][:, :], sc_, None, ALU.mult, None, [b_pb[7]], [bl_[g]])
                    yield
            for g in range(NS):
                pvw = proj_tm_group(wv_i, g, 7)
                cp("dve", VPs[c_][:, 4 * g:4 * g + 4, :], pvw, [b_pb[7]], [B_VP[c_][g]])
                yield

        osb = [AR.alloc([128, 4, 64], F32) for _ in range(2)]
        b_osb = [Buf() for _ in range(2)]
        osq = AR.alloc([128, 4, 64], F32)
        b_osq = Buf()
        ssn = [AR.alloc([128, 4], F32) for _ in range(2)]
        b_ssn = [Buf() for _ in range(2)]
        mixo = [AR.alloc([128, 4, 64], BF16) for _ in range(2)]
        b_mixo = [Buf() for _ in range(2)]
        evr = Rot([0, 1])
        gi_ctr = [0]
        b_wcast = Buf("wcast")
        cast_jobs = []
        for e_i in range(32):
            cast_jobs.append((wgb[e_i * 128:(e_i + 1) * 128, :].rearrange("p (k n) -> p k n", k=8), w_eg[e_i].rearrange("(k p) n -> p k n", p=128)))
            cast_jobs.append((wub[e_i * 128:(e_i + 1) * 128, :].rearrange("p (k n) -> p k n", k=8), w_eu[e_i].rearrange("(k p) n -> p k n", p=128)))
            cast_jobs.append((wdb[e_i * 128:(e_i + 1) * 128, :].rearrange("p (k n) -> p k n", k=4), w_ed[e_i].rearrange("(k p) n -> p k n", p=128)))
        cast_every = max(1, (4 * 2 * (NS * (NS + 1) * 2)) // 100)
        cast_ctr = [0]

        def maybe_cast():
            cast_ctr[0] += 1
            if cast_ctr[0] % cast_every == 0 and cast_jobs:
                o_, i_ = cast_jobs.pop(0)
                dma(o_, i_, [], [], q="pool")
        for _ in proj_pair_gen(0):
            pass
        for pr in range(4):
            qT, kT, Vp = QTs[pr % 2], KTs[pr % 2], VPs[pr % 2]
            b_qT, b_kT, b_Vp = B_QT[pr % 2], B_KT[pr % 2], B_VP[pr % 2]
            nxt_proj = proj_pair_gen(pr + 1) if pr < 3 else None
            units = []
            for hh in range(2):
                for i in range(NS):
                    kbs = list(range(4 * i + 3, -1, -1))
                    for kb in kbs:
                        units.append(dict(hb=hh * 64, head=pr * 2 + hh, i=i, kb=kb, j=kb - 4 * i,
                                          first=(kb == kbs[0]), last=(kb == 0), gi=None))
            gcount = gi_ctr[0]
            for u_ in units:
                if u_["first"]:
                    gcount += 1
                u_["gi"] = gcount
            gi_ctr[0] = gcount
            n = len(units)
            sacc_cur = [0]

            def st0(ix):
                u_ = units[ix]
                hb, i, kb, j = u_["hb"], u_["i"], u_["kb"], u_["j"]
                zi = ix % 2
                diag = j >= 0
                c0 = 128 * max(j, 0)
                mm(pb[zi][:, c0:], kT[hb:hb + 64, kb * 128:(kb + 1) * 128], qT[hb:hb + 64, i * 512 + c0:(i + 1) * 512],
                   True, not diag, [b_kT[kb // 4], b_qT[i]], [b_pb[zi]])
                if diag:
                    mm(pb[zi][:, c0:], idb, dmask[j][:, c0:], False, True, [b_cb], [b_pb[zi]])

            def st1a(ix):
                zi = ix % 2
                c0 = 128 * max(units[ix]["j"], 0)
                actf(Eb[ix % NB][:, c0:], pb[zi][:, c0:], AF.Exp, [b_pb[zi]], [b_E[ix % NB]])

            def st1b(ix):
                c0 = 128 * max(units[ix]["j"], 0)
                actf(Lpb[ix % NB][:, c0:], Eb[ix % NB][:, c0:], AF.Ln, [b_E[ix % NB]], [b_Lp[ix % NB]], bias=1.0)

            def st2(ix):
                u_ = units[ix]
                hb, i, kb, j = u_["hb"], u_["i"], u_["kb"], u_["j"]
                ci = 2 + (ix % 3)
                diag = j >= 0
                lp, blp = Lpb[ix % NB], b_Lp[ix % NB]
                cur = sacc_cur[0]
                c0 = 128 * max(j, 0)
                mm(pb[ci][:, c0:], kT[hb:hb + 64, kb * 128:(kb + 1) * 128], qT[hb:hb + 64, i * 512 + c0:(i + 1) * 512],
                   True, False, [b_kT[kb // 4], b_qT[i]], [b_pb[ci]])
                mm(pb[ci][:, c0:], nuincl, lp[:, c0:], False, (u_["first"] and not diag), [b_cb, blp], [b_pb[ci]])
                if not u_["first"]:
                    mm(pb[ci][:, c0:], nones, Saccs[cur][:, c0:], False, not diag, [b_cb, b_Sacc[cur]], [b_pb[ci]])
                if diag:
                    mm(pb[ci][:, c0:], idb, dmask[j][:, c0:], False, True, [b_cb], [b_pb[ci]])
                if not u_["last"]:
                    nxt = 1 - cur
                    if u_["first"]:
                        P.pool(lambda e, o=Saccs[nxt][:, 0:384]: e.memset(o, 0.0), [], [b_Sacc[nxt]])
                        P.pool(lambda e, o=Saccs[cur][:, 0:256]: e.memset(o, 0.0), [], [b_Sacc[cur]])
                        cp("pool", Saccs[nxt][:, c0:], lp[:, c0:], [blp], [b_Sacc[nxt]])
                    else:
                        tt("pool", Saccs[nxt][:, c0:], Saccs[cur][:, c0:], lp[:, c0:], ALU.add, [b_Sacc[cur], blp], [b_Sacc[nxt]])
                    sacc_cur[0] = nxt

            def st3(ix):
                ci = 2 + (ix % 3)
                c0 = 128 * max(units[ix]["j"], 0)
                actf(ATb[ix % NB][:, c0:], pb[ci][:, c0:], AF.Exp, [b_pb[ci]], [b_AT[ix % NB]])

            def st4(ix):
                u_ = units[ix]
                hb, i, kb, j, head = u_["hb"], u_["i"], u_["kb"], u_["j"], u_["head"]
                obi = 5 + (u_["gi"] % 2)
                Ov = pb[obi][:, 0:256].rearrange("p (s c) -> p s c", s=4)
                at, bat = ATb[ix % NB], b_AT[ix % NB]
                for sub in range(4):
                    if j >= 0 and sub < j:
                        continue
                    P.pe(lambda e, o=Ov[:, sub, :], l=at[:, sub * 128:(sub + 1) * 128], rh=Vp[:, kb, hb:hb + 64],
                         s0=(u_["first"] and sub == 3), s1=(kb == 0 and sub == 3):
                         e.matmul(o, lhsT=l, rhs=rh, start=s0, stop=s1, skip_group_check=True),
                         [bat, b_Vp[kb // 4]], [b_pb[obi]])
                if u_["last"]:
                    gi = u_["gi"]
                    o_, bo = osb[gi % 2], b_osb[gi % 2]
                    sn, bsn = ssn[gi % 2], b_ssn[gi % 2]
                    mo, bmo = mixo[gi % 2], b_mixo[gi % 2]
                    cp("dve", o_, Ov, [b_pb[obi]], [bo])
                    tt("pool", osq, o_, o_, ALU.mult, [bo], [b_osq])
                    P.dve(lambda e, o=sn, i_=osq: e.tensor_reduce(out=o, in_=i_, axis=AX.X, op=ALU.add), [b_osq], [bsn])
                    rstd_from_ss(sn, 64, bsn)
                    tt("dve", o_, o_, sn.unsqueeze(2).to_broadcast([128, 4, 64]), ALU.mult, [bo, bsn], [bo])
                    tt("dve", mo, o_, sbw_b[:, head * 64:(head + 1) * 64].unsqueeze(1).to_broadcast([128, 4, 64]),
                       ALU.mult, [bo, b_sbw], [bmo])
                    dma(mix[i * 512:(i + 1) * 512, head * 64:(head + 1) * 64].rearrange("(s p) c -> p s c", p=128),
                        mo, [bmo], [])

            pf_every = max(1, (n + 3) // (3 * NS + 4))
            for s_ in range(n + 3):
                maybe_cast()
                if nxt_proj is not None and s_ % pf_every == 0:
                    next(nxt_proj, None)
                if s_ < n:
                    st0(s_)
                if 1 <= s_ <= n:
                    st1a(s_ - 1)
                if 3 <= s_ <= n + 2:
                    st3(s_ - 3)
                if 1 <= s_ <= n:
                    st1b(s_ - 1)
                    st2(s_ - 1)
                if 3 <= s_ <= n + 2:
                    st4(s_ - 3)
            if nxt_proj is not None:
                for _ in nxt_proj:
                    pass

        while cast_jobs:
            o_c, i_c = cast_jobs.pop(0)
            dma(o_c, i_c, [], [], q="pool")
        if stop_after == "sb":
            P.emit(nc, st)
            return nc

        qT, kT, Vp = QTs[0], KTs[0], VPs[0]
        b_qT, b_kT, b_Vp = B_QT[0], B_KT[0], B_VP[0]
        P.barrier()
        AR.off = sb_mark
        import os as _os
        _hgstop = int(_os.environ.get("HG_STOP", "0"))

        class _Stop(Exception):
            pass

        def chk(n):
            if _hgstop == n:
                raise _Stop()
        def HG_BODY():
            nonlocal wrot
            eb = AR.alloc([128, S], F32)
            b_eb = [Buf() for _ in range(NS)]
            ktok2 = AR.alloc([128, NT, 128], BF16)
            ktok2h = AR.alloc([128, NT, 128], BF16)
            qTh = AR.alloc([128, S], BF16)
            print('arena after HG big allocs', AR.off, AR.cap)
            b_k2 = [Buf() for _ in range(NS)]
            gw = AR.alloc([128, NT, 128], BF16)
            b_gw = [Buf() for _ in range(NS)]
            t1s = [AR.alloc([128, 512], F32) for _ in range(2)]
            b_t1 = [Buf() for _ in range(2)]
            t2s = [AR.alloc([128, 512], F32) for _ in range(2)]
            b_t2 = [Buf() for _ in range(2)]
            ktk = [AR.alloc([128, 4, 128], F32) for _ in range(2)]
            b_ktk = [Buf() for _ in range(2)]
            ktkb = [AR.alloc([128, 4, 128], BF16) for _ in range(2)]
            b_ktkb = [Buf() for _ in range(2)]
            gtk = [AR.alloc([128, 4, 128], F32) for _ in range(2)]
            b_gtk = [Buf() for _ in range(2)]
            ghis = [AR.alloc([128, 4, 128], BF16) for _ in range(2)]
            glos = [AR.alloc([128, 4, 128], BF16) for _ in range(2)]
            b_gh = [Buf() for _ in range(2)]
            enb = [AR.alloc([128, 512], F32) for _ in range(2)]
            b_enb = [Buf() for _ in range(2)]
            Sst = [AR.alloc([128, 128], F32) for _ in range(4)]
            b_Sst = [Buf() for _ in range(4)]
            Sbf = [AR.alloc([128, 128], BF16) for _ in range(4)]
            b_Sbf = [Buf() for _ in range(4)]
            smb = [AR.alloc([128, 128], BF16) for _ in range(2)]
            b_smb = [Buf() for _ in range(2)]
            ssh = [AR.alloc([128, 1], F32) for _ in range(4)]
            b_ssh = [Buf() for _ in range(4)]
            mixh = [AR.alloc([128, 4, 128], BF16) for _ in range(2)]
            b_mixh = [Buf() for _ in range(2)]
            protP = Rot([6, 7])
            protR = Rot([0, 1, 2, 3, 4, 5])
            wts = {}
            scs = [0]

            def proj_gen(hh, g, tix):
                if g == 0:
                    wts[hh] = (load_w(1536 + hh * 128), load_w(2048 + hh * 128), load_w(2560 + hh * 128), load_w(3072 + hh * 128))
                wq_i, wf_i, wi_i, wg_i = wts[hh]
                hs = slice(hh * 128, (hh + 1) * 128)
                gs = slice(g * 512, (g + 1) * 512)
                t1, bt1 = t1s[tix % 2], b_t1[tix % 2]
                t2, bt2 = t2s[tix % 2], b_t2[tix % 2]
                kt, bkt = ktk[tix % 2], b_ktk[tix % 2]
                ktb, bktb = ktkb[tix % 2], b_ktkb[tix % 2]
                gt, bgt = gtk[tix % 2], b_gtk[tix % 2]
                en, ben = enb[tix % 2], b_enb[tix % 2]
                ghi, glo = ghis[tix % 2], glos[tix % 2]
                bgh = b_gh[tix % 2]
                t1v = t1.rearrange("p (j n) -> p j n", j=4)
                t2v = t2.rearrange("p (j n) -> p j n", j=4)
                bi = protP.next()
                pf = proj_tm_group(wf_i, g, bi)
                actf(t1v, pf, AF.Exp, [b_pb[bi]], [bt1], scale=-1.0)
                yield
                ts("dve", t2, t1, 1.0, None, ALU.add, None, [bt1], [bt2])
                recip_act(t2, t2, [bt2], [bt2])
                tt("dve", t1, t1, t2, ALU.mult, [bt1, bt2], [bt1])
                tt("dve", kt, t1v, oml_b[:, hs].unsqueeze(1).to_broadcast([128, 4, 128]), ALU.mult,
                   [bt1, b_oml], [bkt])
                yield
                actf(gt, kt, AF.Ln, [bkt], [bgt], scale=-1.0, bias=1.0)
                cp("pool", ktb, kt, [bkt], [bktb])
                cp("pool", ghi, gt, [bgt], [bgh])
                tt("dve", glo, gt, ghi, ALU.subtract, [bgt, bgh], [bgh])
                yield
                bi = protP.next()
                pc = pb[bi][:, :].rearrange("p (j n) -> p j n", j=4)
                for j in range(4):
                    mm(pc[:, j, :], ghi[:, j, :], trifwd, True, False, [bgh, b_cb], [b_pb[bi]])
                    mm(pc[:, j, :], glo[:, j, :], trifwd, False, True, [bgh, b_cb], [b_pb[bi]])
                actf(eb[:, gs], pb[bi][:, :], AF.Exp, [b_pb[bi]], [b_eb[g]])
                actf(en, pb[bi][:, :], AF.Exp, [b_pb[bi]], [ben], scale=-1.0)
                yield
                bi = protP.next()
                prv = pb[bi][:, :].rearrange("p (j n) -> p j n", j=4)
                for j in range(4):
                    mm(prv[:, j, :], trirev, ghi[:, j, :], True, False, [bgh, b_cb], [b_pb[bi]])
                    mm(prv[:, j, :], trirev, glo[:, j, :], False, True, [bgh, b_cb], [b_pb[bi]])
                actf(t2, pb[bi][:, :], AF.Exp, [b_pb[bi]], [bt2])
                yield
                stt("dve", ktok2[:, 4 * g:4 * g + 4, :], kt, pm0, t2v, ALU.mult, ALU.mult, [bkt, bt2, b_cb], [b_k2[g]])
                stt("dve", ktok2h[:, 4 * g:4 * g + 4, :], kt, pm1, t2v, ALU.mult, ALU.mult, [bkt, bt2, b_cb], [b_k2[g]])
                yield
                bi = protP.next()
                pk = bf16view(pb[bi])[:, 0:512].rearrange("p (j n) -> p j n", j=4)
                for j in range(4):
                    tr(pk[:, j, :], ktb[:, j, :], idb, [bktb, b_cb], [b_pb[bi]])
                tt("dve", kT[:, gs], bf16view(pb[bi])[:, 0:512], en, ALU.mult, [b_pb[bi], ben], [b_kT[g]])
                yield
                bi = protP.next()
                for k in range(8):
                    mm(pb[bi][:, :], wsl[wq_i][:, k, :], aT[:, k, gs], k == 0, k == 7, aT_bufs(g) + [b_wsl[wq_i]], [b_pb[bi]])
                actf(t1, pb[bi][:, :], AF.Exp, [b_pb[bi]], [bt1], scale=-1.0)
                ts("dve", t1, t1, 1.0, None, ALU.add, None, [bt1], [bt1])
                recip_act(t1, t1, [bt1], [bt1])
                tt("dve", t1, pb[bi][:, :], t1, ALU.mult, [b_pb[bi], bt1], [bt1])
                yield
                tt("dve", t1, t1, eb[:, gs], ALU.mult, [bt1, b_eb[g]], [bt1])
                tt("dve", qT[:, gs], t1, cm0, ALU.mult, [bt1, b_cb], [b_qT[g]])
                tt("dve", qTh[:, gs], t1, cm1, ALU.mult, [bt1, b_cb], [b_qT[g]])
                yield
                bi = protP.next()
                pvw = proj_tm_group(wi_i, g, bi)
                cp("act", Vp[:, 4 * g:4 * g + 4, :], pvw, [b_pb[bi]], [b_Vp[g]])
                yield
                bi = protP.next()
                pg = proj_tm_group(wg_i, g, bi)
                actf(t2v, pg, AF.Exp, [b_pb[bi]], [bt2], scale=-1.0)
                ts("dve", t2, t2, 1.0, None, ALU.add, None, [bt2], [bt2])
                recip_act(t2, t2, [bt2], [bt2])
                tt("dve", t2v, pg, t2v, ALU.mult, [b_pb[bi], bt2], [bt2])
                yield
                tt("dve", gw[:, 4 * g:4 * g + 4, :], t2v, hgw_b[:, hs].unsqueeze(1).to_broadcast([128, 4, 128]), ALU.mult,
                   [bt2, b_hgw], [b_gw[g]])
                yield

            def rec_gen(hh, g):
                if g == 0:
                    scs[0] = 0
                    P.dve(lambda e, o=Sst[0]: e.memset(o, 0.0), [], [b_Sst[0]])
                    P.dve(lambda e, o=Sbf[0]: e.memset(o, 0.0), [], [b_Sbf[0]])
                for t in range(4 * g, 4 * g + 4):
                    sc = scs[0]
                    tsl = slice(t * 128, (t + 1) * 128)
                    bs = protR.next()
                    mm(pb[bs][:, 0:128], kT[:, tsl], qT[:, tsl], True, False, [b_kT[g], b_qT[g]], [b_pb[bs]])
                    mm(pb[bs][:, 0:128], kT[:, tsl], qTh[:, tsl], False, True, [b_kT[g], b_qT[g]], [b_pb[bs]])
                    bu = protR.next()
                    Uv = pb[bu][:, 0:256].rearrange("p (j n) -> p j n", j=2)
                    mm(Uv[:, 0, :], ktok2[:, t, :], Vp[:, t, :], True, True, [b_k2[g], b_Vp[g]], [b_pb[bu]])
                    mm(Uv[:, 1, :], ktok2h[:, t, :], Vp[:, t, :], True, True, [b_k2[g], b_Vp[g]], [b_pb[bu]])
                    sm, bsm = smb[t % 2], b_smb[t % 2]
                    tt("dve", sm, pb[bs][:, 0:128], trifwd, ALU.mult, [b_pb[bs], b_cb], [bsm])
                    s0, s1, s2 = sc % 4, (sc + 1) % 4, (sc + 2) % 4
                    c0 = 2 * t
                    stt("dve", Sst[s1], Sst[s0], eb[:, c0 * 64 + 63:c0 * 64 + 64], Uv[:, 0, :], ALU.mult, ALU.add,
                        [b_Sst[s0], b_eb[g], b_pb[bu]], [b_Sst[s1]])
                    cp("pool", Sbf[s1], Sst[s1], [b_Sst[s1]], [b_Sbf[s1]])
                    c1 = 2 * t + 1
                    stt("dve", Sst[s2], Sst[s1], eb[:, c1 * 64 + 63:c1 * 64 + 64], Uv[:, 1, :], ALU.mult, ALU.add,
                        [b_Sst[s1], b_eb[g], b_pb[bu]], [b_Sst[s2]])
                    cp("pool", Sbf[s2], Sst[s2], [b_Sst[s2]], [b_Sbf[s2]])
                    scs[0] = sc + 2
                    yield
                    bo_ = protR.next()
                    Op_ = pb[bo_][:, 0:128]
                    mm(Op_, sm, Vp[:, t, :], True, False, [bsm, b_Vp[g]], [b_pb[bo_]])
                    mm(Op_, qT[:, tsl], Sbf[s0], False, False, [b_qT[g], b_Sbf[s0]], [b_pb[bo_]])
                    mm(Op_, qTh[:, tsl], Sbf[s1], False, True, [b_qT[g], b_Sbf[s1]], [b_pb[bo_]])
                    sh, bsh = ssh[t % 4], b_ssh[t % 4]
                    actf(junk[:, 0:128], Op_, AF.Square, [b_pb[bo_]], [b_junk, bsh], accum=sh)
                    rstd_from_ss(sh, 128, bsh)
                    yield
                    mh, bmh = mixh[(t // 4) % 2], b_mixh[(t // 4) % 2]
                    stt("dve", mh[:, t % 4, :], Op_, sh, gw[:, t, :], ALU.mult, ALU.mult, [b_pb[bo_], bsh, b_gw[g]], [bmh])
                    if t % 4 == 3:
                        dma(mix[(t - 3) * 128:(t + 1) * 128, 512 + hh * 128:512 + (hh + 1) * 128].rearrange("(j p) c -> p j c", p=128),
                            mh, [bmh], [])
                    yield

            stages = [(hh, g) for hh in range(4) for g in range(NS)]
            prev = None
            for k_, (hh, g) in enumerate(stages):
                gens = [proj_gen(hh, g, k_)]
                if prev is not None:
                    gens.append(prev)
                run_interleaved(gens, 2)
                prev = rec_gen(hh, g)
            run_interleaved([prev], 1)

        try:
            HG_BODY()
        except _Stop:
            P.emit(nc, st)
            return nc
        if stop_after == "hg":
            P.emit(nc, st)
            return nc
        P.barrier()
        AR.off = base_mark
        NQ = 2 * NT
        NSLOT = NQ + 64
        RROWS = NSLOT * 128
        OOBV = 1 << 20
        b_msc = Buf("m_scr")
        b_h1sc = Buf("h1_scr")
        b_ysc = Buf("y_scr")
        b_lst = Buf("lst")
        finw_b = AR.alloc([128, D], F32)
        b_finw = Buf()
        dma(finw_b, finw.partition_broadcast(128), [], [b_finw])
        plwT = AR.alloc([128, 8], F32)
        b_nw2 = Buf()
        dma(plwT, plw, [], [b_nw2])
        junkB = AR.alloc([128, D], F32)
        b_junkB = Buf()
        OHall = AR.alloc([128, NQ, 32], BF16)
        b_OH = [Buf() for _ in range(NQ)]
        gall = AR.alloc([128, NQ], F32)
        b_gall = [Buf() for _ in range(NQ)]
        pass_mark = AR.off
        prot = Rot(range(8))
        fnw_b = AR.alloc([128, D], F32)
        b_fnwb = Buf()
        dma(fnw_b, fnwv.partition_broadcast(128), [], [b_fnwb])
        br_b = AR.alloc([128, 36], F32)
        b_br = Buf()
        dma(br_b, b_r.partition_broadcast(128), [], [b_br])
        wr32 = AR.alloc([128, 8, 36], F32)
        b_wr = Buf()
        dma(wr32, w_r.rearrange("(k p) n -> p k n", p=128), [], [b_wr])
        wrhi = AR.alloc([128, 8, 36], BF16)
        wrlo = AR.alloc([128, 8, 36], BF16)
        cp("dve", wrhi, wr32, [b_wr], [b_wr])
        tt("dve", wrlo, wr32, wrhi, ALU.subtract, [b_wr], [b_wr])
        wo = AR.alloc([128, 8, D], BF16)
        b_wo = Buf()
        dma(wo, w_out.rearrange("(k p) n -> p k n", p=128), [], [b_wo], q="pool")
        xts = [AR.alloc([128, D], F32) for _ in range(WB)]
        b_xts = [Buf() for _ in range(WB)]
        mts = [AR.alloc([128, D], BF16) for _ in range(WB)]
        b_mts = [Buf() for _ in range(WB)]
        mxT = [AR.alloc([128, 8, 128], BF16) for _ in range(WB)]
        b_mxT = [Buf() for _ in range(WB)]
        h1t = [AR.alloc([128, D], F32) for _ in range(WB)]
        b_h1t = [Buf() for _ in range(WB)]
        mn32 = [AR.alloc([128, D], F32) for _ in range(WB)]
        b_mn32 = [Buf() for _ in range(WB)]
        mhi = [AR.alloc([128, D], BF16) for _ in range(WB)]
        b_mhi = [Buf() for _ in range(WB)]
        mlo = [AR.alloc([128, D], BF16) for _ in range(WB)]
        b_mlo = [Buf() for _ in range(WB)]
        mhiT = [AR.alloc([128, 8, 128], BF16) for _ in range(WB)]
        b_mhiT = [Buf() for _ in range(WB)]
        mloT = [AR.alloc([128, 8, 128], BF16) for _ in range(WB)]
        b_mloT = [Buf() for _ in range(WB)]
        rt = [AR.alloc([128, 160], F32) for _ in range(WB)]
        b_rt = [Buf() for _ in range(WB)]
        def b1_tile(t):
            s_ = t % WB
            xt, bx = xts[s_], b_xts[s_]
            mt, bm = mts[s_], b_mts[s_]
            dma(xt, x[t * 128:(t + 1) * 128, :], [], [bx])
            dma(mt, mix[t * 128:(t + 1) * 128, :], [], [bm])
            bi = prot.next()
            pv = bf16view(pb[bi]).rearrange("p (k n) -> p k n", k=8)
            for k in range(8):
                tr(pv[:, k, :], mt[:, k * 128:(k + 1) * 128], idb, [bm, b_cb], [b_pb[bi]])
            cp("act", mxT[s_], pv, [b_pb[bi]], [b_mxT[s_]])
            yield
            h1, bh1 = h1t[s_], b_h1t[s_]
            for half in range(2):
                hsl = slice(half * 512, (half + 1) * 512)
                bi = prot.next()
                for k in range(8):
                    mm(pb[bi][:, :], mxT[s_][:, k, :], wo[:, k, hsl], k == 0, k == 7, [b_mxT[s_], b_wo], [b_pb[bi]])
                tt("dve", h1[:, hsl], pb[bi][:, :], xt[:, hsl], ALU.add, [b_pb[bi], bx], [bh1])
            dma(h1_scr[t * 128:(t + 1) * 128, :], h1, [bh1], [])
            r_, br_ = rt[s_], b_rt[s_]
            yield
            ss1 = r_[:, 0:1]
            actf(junkB, h1, AF.Square, [bh1], [b_junkB, br_], accum=ss1)
            rstd_from_ss(ss1, D, br_)
            yield
            stt("dve", mn32[s_], h1, ss1, fnw_b, ALU.mult, ALU.mult, [bh1, br_, b_fnwb], [b_mn32[s_]])
            yield
            cp("act", mhi[s_], mn32[s_], [b_mn32[s_]], [b_mhi[s_]])
            yield
            tt("dve", mlo[s_], mn32[s_], mhi[s_], ALU.subtract, [b_mn32[s_], b_mhi[s_]], [b_mlo[s_]])
            yield
            dma(m_scr[t * 128:(t + 1) * 128, :], mhi[s_], [b_mhi[s_]], [])
            dma(m_scr[S + t * 128:S + (t + 1) * 128, :], mhi[s_], [b_mhi[s_]], [])
            bi = prot.next()
            pvh = bf16view(pb[bi]).rearrange("p (k n) -> p k n", k=8)
            for k in range(8):
                tr(pvh[:, k, :], mhi[s_][:, k * 128:(k + 1) * 128], idb, [b_mhi[s_], b_cb], [b_pb[bi]])
            cp("act", mhiT[s_], pvh, [b_pb[bi]], [b_mhiT[s_]])
            yield
            bi = prot.next()
            pvl = bf16view(pb[bi]).rearrange("p (k n) -> p k n", k=8)
            for k in range(8):
                tr(pvl[:, k, :], mlo[s_][:, k * 128:(k + 1) * 128], idb, [b_mlo[s_], b_cb], [b_pb[bi]])
            cp("dve", mloT[s_], pvl, [b_pb[bi]], [b_mloT[s_]])
            yield
            bi = prot.next()
            for k in range(8):
                mm(pb[bi][:, 0:36], mhiT[s_][:, k, :], wrhi[:, k, :], k == 0, False, [b_mhiT[s_], b_wr], [b_pb[bi]])
                mm(pb[bi][:, 0:36], mloT[s_][:, k, :], wrhi[:, k, :], False, False, [b_mloT[s_], b_wr], [b_pb[bi]])
                mm(pb[bi][:, 0:36], mhiT[s_][:, k, :], wrlo[:, k, :], False, k == 7, [b_mhiT[s_], b_wr], [b_pb[bi]])
            lg = r_[:, 8:44]
            gmax = r_[:, 1:2]
            ngmax = r_[:, 2:3]
            sg_ = r_[:, 3:4]
            gprob = r_[:, 4:5]
            v1 = r_[:, 5:6]
            v2 = r_[:, 6:7]
            dd = r_[:, 7:8]
            oh = r_[:, 44:48]
            pen = r_[:, 48:52]
            egj = r_[:, 52:56]
            elm = r_[:, 56:88]
            is1 = r_[:, 88:120]
            is2 = r_[:, 120:152]
            e2 = r_[:, 152:153]
            p1 = r_[:, 153:154]
            p2 = r_[:, 154:155]
            R = [br_]
            tt("dve", lg, pb[bi][:, 0:36], br_b, ALU.add, [b_pb[bi], b_br], R)
            yield
            P.dve(lambda e, o=gmax, i_=lg[:, 0:4]: e.tensor_reduce(out=o, in_=i_, axis=AX.X, op=ALU.max), R, R)
            ts("dve", oh, lg[:, 0:4], gmax, None, ALU.is_equal, None, R, R)
            ts("dve", ngmax, gmax, -1.0, None, ALU.mult, None, R, R)
            actf(egj, lg[:, 0:4], AF.Exp, R, R, bias=ngmax, accum=sg_)
            yield
            recip(gprob, sg_, R, R)
            ts("dve", pen, oh, 1.0, 1e30, ALU.subtract, ALU.mult, R, R)
            tt("dve", elm.rearrange("p (g e) -> p g e", g=4), lg[:, 4:36].rearrange("p (g e) -> p g e", g=4),
               pen.unsqueeze(2).to_broadcast([128, 4, 8]), ALU.add, R, R)
            P.dve(lambda e, o=v1, i_=elm: e.tensor_reduce(out=o, in_=i_, axis=AX.X, op=ALU.max), R, R)
            ts("dve", is1, elm, v1, None, ALU.is_equal, None, R, R)
            stt("dve", elm, is1, -1e30, elm, ALU.mult, ALU.add, R, R)
            P.dve(lambda e, o=v2, i_=elm: e.tensor_reduce(out=o, in_=i_, axis=AX.X, op=ALU.max), R, R)
            ts("dve", is2, elm, v2, None, ALU.is_equal, None, R, R)
            tt("dve", dd, v2, v1, ALU.subtract, R, R)
            actf(e2, dd, AF.Exp, R, R)
            yield
            ts("dve", p1, e2, 1.0, None, ALU.add, None, R, R)
            recip(p1, p1, R, R)
            tt("dve", p2, e2, p1, ALU.mult, R, R)
            tt("dve", gall[:, t:t + 1], p1, gprob, ALU.mult, R, [b_gall[t]])
            tt("dve", gall[:, NT + t:NT + t + 1], p2, gprob, ALU.mult, R, [b_gall[NT + t]])
            cp("dve", OHall[:, t, :], is1, R, [b_OH[t]])
            cp("dve", OHall[:, NT + t, :], is2, R, [b_OH[NT + t]])
        run_interleaved([b1_tile(t) for t in range(NT)], WB)
        if stop_after == "b1":
            P.emit(nc, st)
            return nc
        P.barrier()
        AR.off = pass_mark
        Cacc = [AR.alloc([128, 32], BF16) for _ in range(2)]
        b_Cacc = [Buf() for _ in range(2)]
        rk = AR.alloc([128, NQ], F32)
        b_rk = Buf()
        tmp32 = [AR.alloc([128, 32], F32) for _ in range(2)]
        b_tmp32 = [Buf() for _ in range(2)]
        P.dve(lambda e, o=Cacc[0]: e.memset(o, 0.0), [], [b_Cacc[0]])
        for q in range(NQ):
            bi = prot.next()
            cc = q % 2
            mm(pb[bi][:, 0:32], pones, OHall[:, q, :], True, False, [b_cb, b_OH[q]], [b_pb[bi]])
            mm(pb[bi][:, 0:32], nuincl, OHall[:, q, :], False, q == 0, [b_cb, b_OH[q]], [b_pb[bi]])
            if q > 0:
                mm(pb[bi][:, 0:32], pones, Cacc[cc], False, True, [b_cb, b_Cacc[cc]], [b_pb[bi]])
            tt("dve", tmp32[cc], pb[bi][:, 0:32], OHall[:, q, :], ALU.mult, [b_pb[bi], b_OH[q]], [b_tmp32[cc]])
            P.dve(lambda e, o=rk[:, q:q + 1], i_=tmp32[cc]: e.tensor_reduce(out=o, in_=i_, axis=AX.X, op=ALU.add), [b_tmp32[cc]], [b_rk])
            tt("pool", Cacc[1 - cc], Cacc[cc], OHall[:, q, :], ALU.add, [b_Cacc[cc], b_OH[q]], [b_Cacc[1 - cc]])
        cfin = NQ % 2
        bi = prot.next()
        mm(pb[bi][:, 0:32], pones, Cacc[cfin], True, True, [b_cb, b_Cacc[cfin]], [b_pb[bi]])
        ntot = AR.alloc([128, 32], F32)
        nti = AR.alloc([128, 32], I32)
        npad = AR.alloc([128, 32], F32)
        offs = AR.alloc([128, 33], F32)
        b_off = Buf()
        ts("dve", ntot, pb[bi][:, 0:32], 255.0, None, ALU.add, None, [b_pb[bi]], [b_off])
        cp("dve", nti, ntot, [b_off], [b_off])
        P.dve(lambda e, o=nti: e.tensor_single_scalar(out=o, in_=o, scalar=8, op=ALU.arith_shift_right), [b_off], [b_off])
        P.dve(lambda e, o=nti: e.tensor_single_scalar(out=o, in_=o, scalar=8, op=ALU.logical_shift_left), [b_off], [b_off])
        cp("dve", npad, nti, [b_off], [b_off])
        P.dve(lambda e, o=offs[:, 0:1]: e.memset(o, 0.0), [], [b_off])
        for e_i in range(32):
            tt("dve", offs[:, e_i + 1:e_i + 2], offs[:, e_i:e_i + 1], npad[:, e_i:e_i + 1], ALU.add, [b_off], [b_off])
        posf = AR.alloc([128, NQ], F32)
        posi = AR.alloc([128, NQ], I32)
        b_pos = Buf()
        for q in range(NQ):
            cc = q % 2
            tt("dve", tmp32[cc], offs[:, 0:32], OHall[:, q, :], ALU.mult, [b_off, b_OH[q]], [b_tmp32[cc]])
            P.dve(lambda e, o=posf[:, q:q + 1], i_=tmp32[cc]: e.tensor_reduce(out=o, in_=i_, axis=AX.X, op=ALU.add), [b_tmp32[cc]], [b_pos])
        tt("dve", posf, posf, rk, ALU.add, [b_pos, b_rk], [b_pos])
        cp("dve", posi, posf, [b_pos], [b_pos])
        hi_f = AR.alloc([128, NQ], F32)
        P.dve(lambda e, o=posi: e.tensor_single_scalar(out=o, in_=o, scalar=7, op=ALU.arith_shift_right), [b_pos], [b_pos])
        cp("dve", hi_f, posi, [b_pos], [b_pos])
        stt("dve", posf, hi_f, -128.0, posf, ALU.mult, ALU.add, [b_pos], [b_pos])
        stt("dve", posf, posf, float(NSLOT), hi_f, ALU.mult, ALU.add, [b_pos], [b_pos])
        cp("dve", posi, posf, [b_pos], [b_pos])
        pidt = AR.alloc([128, NQ], I32)
        b_pidt = Buf()
        dma(pidt, pairid_d, [], [b_pidt])
        linit = AR.alloc([128, NSLOT], I32)
        b_linit = Buf()
        dma(linit, linit_d, [], [b_linit])
        dma(lst_i.rearrange("(p s) c -> p (s c)", p=128), linit, [b_linit], [b_lst])
        for q in range(NQ):
            P.dma(lambda e, q=q: e.indirect_dma_start(out=lst_i[:, :], out_offset=bass.IndirectOffsetOnAxis(ap=posi[:, q:q + 1], axis=0),
                  in_=pidt[:, q:q + 1], in_offset=None, bounds_check=None),
                  [b_pos, b_pidt, b_lst], [], q="pool")
            P.dma(lambda e, q=q: e.indirect_dma_start(out=lst_g[:, :], out_offset=bass.IndirectOffsetOnAxis(ap=posi[:, q:q + 1], axis=0),
                  in_=gall[:, q:q + 1], in_offset=None, bounds_check=None),
                  [b_pos, b_gall[q], b_lst], [], q="pool")
        lsb_i = AR.alloc([128, NSLOT], I32)
        lsb_g = AR.alloc([128, NSLOT], F32)
        b_lsb = Buf()
        dma(lsb_i, lst_i.rearrange("(p s) c -> p (s c)", p=128), [], [b_lsb, b_lst])
        dma(lsb_g, lst_g.rearrange("(p s) c -> p (s c)", p=128), [], [b_lsb, b_lst])
        sst = AR.alloc([128, NSLOT], F32)
        sef = AR.alloc([128, NSLOT], F32)
        widx = AR.alloc([128, NSLOT], I32)
        piota = AR.alloc([128, 1], F32)
        b_se = Buf()
        dma(sst, sstart_d, [], [b_se])
        dma(piota, piota_d, [], [b_se])
        P.dve(lambda e, o=sef: e.memset(o, 0.0), [], [b_se])
        for e_i in range(32):
            stt("dve", sef, sst, offs[:, e_i + 1:e_i + 2], sef, ALU.is_ge, ALU.add, [b_se, b_off], [b_se])
        ts("dve", sef, sef, 31.0, None, ALU.min, None, [b_se], [b_se])
        ts("dve", sef, sef, 128.0, None, ALU.mult, None, [b_se], [b_se])
        ts("dve", sef, sef, piota, None, ALU.add, None, [b_se], [b_se])
        cp("dve", widx, sef, [b_se], [b_se])
        sort_mark = AR.off
        if stop_after == "sort":
            P.emit(nc, st)
            return nc
        NBUF = 3
        Wg = [AR.alloc([128, 4096], BF16) for _ in range(NBUF)]
        Wu = [AR.alloc([128, 4096], BF16) for _ in range(NBUF)]
        Wd = [AR.alloc([128, 4096], BF16) for _ in range(NBUF)]
        b_W = [Buf() for _ in range(NBUF)]
        xg = [AR.alloc([128, D], BF16) for _ in range(2 * NBUF)]
        b_xg = [Buf() for _ in range(2 * NBUF)]
        xgT = [AR.alloc([128, 8, 128], BF16) for _ in range(2)]
        b_xgT = [Buf() for _ in range(2)]
        sgs = [AR.alloc([128, 512], F32) for _ in range(2)]
        b_sgs = [Buf() for _ in range(2)]
        hs_ = [AR.alloc([128, 512], BF16) for _ in range(2)]
        b_hs = [Buf() for _ in range(2)]
        hTs = [AR.alloc([128, 4, 128], BF16) for _ in range(2)]
        b_hTs = [Buf() for _ in range(2)]
        ysb = [AR.alloc([128, D], F32) for _ in range(2)]
        b_ysb = [Buf() for _ in range(2)]
        for xi_, xg_ in enumerate(xg):
            P.dve(lambda e, o=xg_: e.memset(o, 0.0), [], [b_xg[xi_]])

        def igather(dst, src, idx_ap, bound, r, w):
            P.dma(lambda e, dst=dst, src=src, idx_ap=idx_ap, bound=bound: e.indirect_dma_start(
                out=dst, out_offset=None, in_=src, in_offset=bass.IndirectOffsetOnAxis(ap=idx_ap, axis=0),
                bounds_check=None), r, w, q="pool")

        def slot_gathers(J):
            s_ = J % NBUF
            for h_ in range(2):
                j = 2 * J + h_
                xi = 2 * s_ + h_
                igather(xg[xi][:, :], m_scr[:, :], lsb_i[:, j:j + 1], 2 * S - 1, [b_lsb, b_xg[xi]], [b_xg[xi]])
            igather(Wg[s_][:, :], wgb[:, :], widx[:, 2 * J:2 * J + 1], 32 * 128 - 1, [b_se], [b_W[s_]])
            igather(Wu[s_][:, :], wub[:, :], widx[:, 2 * J:2 * J + 1], 32 * 128 - 1, [b_se], [b_W[s_]])
            igather(Wd[s_][:, :], wdb[:, :], widx[:, 2 * J:2 * J + 1], 32 * 128 - 1, [b_se], [b_W[s_]])

        def slot_compute(j):
            s_ = (j // 2) % NBUF
            d_ = j % 2
            xi = 2 * s_ + d_
            bi = prot.next()
            pv = bf16view(pb[bi]).rearrange("p (k n) -> p k n", k=8)
            for k in range(8):
                tr(pv[:, k, :], xg[xi][:, k * 128:(k + 1) * 128], idb, [b_xg[xi], b_cb], [b_pb[bi]])
            cp("act", xgT[d_], pv, [b_pb[bi]], [b_xgT[d_]])
            bg = prot.next()
            for k in range(8):
                mm(pb[bg][:, :], xgT[d_][:, k, :], Wg[s_][:, k * 512:(k + 1) * 512], k == 0, k == 7, [b_xgT[d_], b_W[s_]], [b_pb[bg]])
            bu = prot.next()
            for k in range(8):
                mm(pb[bu][:, :], xgT[d_][:, k, :], Wu[s_][:, k * 512:(k + 1) * 512], k == 0, k == 7, [b_xgT[d_], b_W[s_]], [b_pb[bu]])
            actf(sgs[d_], pb[bg][:, :], AF.Silu, [b_pb[bg]], [b_sgs[d_]])
            tt("dve", hs_[d_], sgs[d_], pb[bu][:, :], ALU.mult, [b_sgs[d_], b_pb[bu]], [b_hs[d_]])
            bi = prot.next()
            ph = bf16view(pb[bi])[:, 0:512].rearrange("p (k n) -> p k n", k=4)
            for k in range(4):
                tr(ph[:, k, :], hs_[d_][:, k * 128:(k + 1) * 128], idb, [b_hs[d_], b_cb], [b_pb[bi]])
            cp("act", hTs[d_], ph, [b_pb[bi]], [b_hTs[d_]])
            gate_ap = lsb_g[:, j:j + 1]
            for half in range(2):
                by = prot.next()
                for fc in range(4):
                    mm(pb[by][:, :], hTs[d_][:, fc, :], Wd[s_][:, fc * 1024 + half * 512:fc * 1024 + (half + 1) * 512],
                       fc == 0, fc == 3, [b_hTs[d_], b_W[s_]], [b_pb[by]])
                ts("dve", ysb[d_][:, half * 512:(half + 1) * 512], pb[by][:, :], gate_ap, None, ALU.mult, None,
                   [b_pb[by], b_lsb], [b_ysb[d_]])
            P.dma(lambda e, j=j, d_=d_: e.indirect_dma_start(out=y_scr[:, :], out_offset=bass.IndirectOffsetOnAxis(ap=lsb_i[:, j:j + 1], axis=0),
                  in_=ysb[d_][:, :], in_offset=None, bounds_check=None),
                  [b_lsb, b_ysb[d_]], [], q="pool")

        NBIG = NSLOT // 2
        slot_gathers(0)
        slot_gathers(1)
        for J in range(NBIG):
            if J + 2 < NBIG:
                slot_gathers(J + 2)
            slot_compute(2 * J)
            slot_compute(2 * J + 1)
        if stop_after == "slots":
            P.emit(nc, st)
            return nc
        P.barrier()
        AR.off = pass_mark
        wpg = AR.alloc([128, 8, D], BF16)
        wpp = AR.alloc([128, 2, D], BF16)
        b_wp = Buf()
        dma(wpg, w_pg.rearrange("(k p) n -> p k n", p=128), [], [b_wp], q="pool")
        dma(wpp, w_pp.rearrange("(k p) n -> p k n", p=128), [], [b_wp], q="pool")
        pts = [AR.alloc([128, 256], F32) for _ in range(WB)]
        b_pts = [Buf() for _ in range(WB)]
        ptb = [AR.alloc([128, 256], BF16) for _ in range(WB)]
        b_ptb = [Buf() for _ in range(WB)]
        ppT = [AR.alloc([128, 2, 128], BF16) for _ in range(WB)]
        b_ppT = [Buf() for _ in range(WB)]
        h2s = [AR.alloc([128, D], F32) for _ in range(WB)]
        b_h2s = [Buf() for _ in range(WB)]
        y0s = [AR.alloc([128, D], F32) for _ in range(WB)]
        y1s = [AR.alloc([128, D], F32) for _ in range(WB)]
        b_ys = [Buf() for _ in range(WB)]
        h2b = [AR.alloc([128, D], BF16) for _ in range(WB)]
        b_h2b = [Buf() for _ in range(WB)]
        h2T = [AR.alloc([128, 8, 128], BF16) for _ in range(WB)]
        b_h2T = [Buf() for _ in range(WB)]
        gts = [AR.alloc([128, D], F32) for _ in range(WB)]
        b_gts = [Buf() for _ in range(WB)]
        h3s = [AR.alloc([128, D], F32) for _ in range(WB)]
        b_h3s = [Buf() for _ in range(WB)]
        ots = [AR.alloc([128, D], F32) for _ in range(WB)]
        b_ots = [Buf() for _ in range(WB)]
        r3 = [AR.alloc([128, 4], F32) for _ in range(WB)]
        b_r3 = [Buf() for _ in range(WB)]
        def b3_tile(t):
            s_ = t % WB
            h2 = h2s[s_]
            bh2 = b_h2s[s_]
            dma(h2, h1_scr[t * 128:(t + 1) * 128, :], [], [bh2])
            dma(y0s[s_], y_scr[t * 128:(t + 1) * 128, :], [], [b_ys[s_]])
            dma(y1s[s_], y_scr[S + t * 128:S + (t + 1) * 128, :], [], [b_ys[s_]])
            dma(pts[s_], p_in[t * 128:(t + 1) * 128, :], [], [b_pts[s_]])
            tt("pool", y0s[s_], y0s[s_], y1s[s_], ALU.add, [b_ys[s_]], [b_ys[s_]])
            tt("dve", h2, h2, y0s[s_], ALU.add, [bh2, b_ys[s_]], [bh2])
            yield
            ss2 = r3[s_][:, 0:1]
            nrs2 = r3[s_][:, 1:2]
            ss3 = r3[s_][:, 2:3]
            R3 = [b_r3[s_]]
            actf(junkB, h2, AF.Square, [bh2], [b_junkB] + R3, accum=ss2)
            rstd_from_ss(ss2, D, b_r3[s_])
            yield
            ts("dve", nrs2, ss2, -1.0, None, ALU.mult, None, R3, R3)
            cp("act", h2b[s_], h2, [bh2], [b_h2b[s_]])
            yield
            bi = prot.next()
            pv = bf16view(pb[bi]).rearrange("p (k n) -> p k n", k=8)
            for k in range(8):
                tr(pv[:, k, :], h2b[s_][:, k * 128:(k + 1) * 128], idb, [b_h2b[s_], b_cb], [b_pb[bi]])
            tt("dve", h2T[s_], pv, plwT.unsqueeze(2).to_broadcast([128, 8, 128]), ALU.mult, [b_pb[bi], b_nw2], [b_h2T[s_]])
            yield
            cp("pool", ptb[s_], pts[s_], [b_pts[s_]], [b_ptb[s_]])
            yield
            bi = prot.next()
            pv2 = bf16view(pb[bi]).rearrange("p (k n) -> p k n", k=8)
            for k in range(2):
                tr(pv2[:, k, :], ptb[s_][:, k * 128:(k + 1) * 128], idb, [b_ptb[s_], b_cb], [b_pb[bi]])
            cp("act", ppT[s_], pv2[:, 0:2, :], [b_pb[bi]], [b_ppT[s_]])
            yield
            for half in range(2):
                hsl = slice(half * 512, (half + 1) * 512)
                bg = prot.next()
                for k in range(8):
                    mm(pb[bg][:, :], h2T[s_][:, k, :], wpg[:, k, hsl], k == 0, k == 7, [b_h2T[s_], b_wp], [b_pb[bg]])
                be = prot.next()
                for k in range(2):
                    mm(pb[be][:, :], ppT[s_][:, k, :], wpp[:, k, hsl], k == 0, k == 1, [b_ppT[s_], b_wp], [b_pb[be]])
                gt_ = gts[s_][:, hsl]
                actf(gt_, pb[bg][:, :], AF.Exp, [b_pb[bg]] + R3, [b_gts[s_]], scale=nrs2)
                ts("dve", gt_, gt_, 1.0, None, ALU.add, None, [b_gts[s_]], [b_gts[s_]])
                recip_act(gt_, gt_, [b_gts[s_]], [b_gts[s_]])
                tt("dve", gt_, gt_, pb[be][:, :], ALU.mult, [b_gts[s_], b_pb[be]], [b_gts[s_]])
                tt("pool", h3s[s_][:, hsl], gt_, h2[:, hsl], ALU.add, [b_gts[s_], bh2], [b_h3s[s_]])
                yield
            actf(junkB, h3s[s_], AF.Square, [b_h3s[s_]], [b_junkB] + R3, accum=ss3)
            rstd_from_ss(ss3, D, b_r3[s_])
            yield
            stt("dve", ots[s_], h3s[s_], ss3, finw_b, ALU.mult, ALU.mult, [b_h3s[s_], b_finw] + R3, [b_ots[s_]])
            dma(out[t * 128:(t + 1) * 128, :], ots[s_], [b_ots[s_]], [])
        run_interleaved([b3_tile(t) for t in range(NT)], WB)
        P.emit(nc, st)
    return nc


def _consts():
    cb = np.zeros((128, NCB), np.float32)
    j = np.arange(128)[:, None]
    s = np.arange(128)[None, :]
    cb[:, 0:128] = np.eye(128, dtype=np.float32)
    cb[:, 128:256] = np.where(j >= s, -1.0, 0.0)
    cb[:, 256:384] = -1.0
    c = np.arange(512)[None, :]
    for jj in range(4):
        cb[:, 640 + 512 * jj:640 + 512 * (jj + 1)] = np.where(c > j + 128 * jj, 0.0, -30000.0)
    cb[:64, 2688] = 1.0
    cb[64:, 2689] = 1.0
    cc = np.arange(512)
    cb[:, 2696:2696 + 512] = ((cc % 128) < 64).astype(np.float32)[None, :]
    cb[:, 2696 + 512:2696 + 1024] = ((cc % 128) >= 64).astype(np.float32)[None, :]
    cb[:, 3720:3848] = 1.0
    cf = np.zeros((128, NCF), np.float32)
    same = (j // 64) == (s // 64)
    cb[:, 384:512] = np.where((j <= s) & same, 1.0, 0.0)
    cb[:, 512:640] = np.where((j > s) & same, 1.0, 0.0)
    return cb, cf


def make_in_maps(inputs, S, n):
    f = lambda a: np.ascontiguousarray(np.asarray(a, dtype=np.float32))
    cb, cf = _consts()
    shared = {
        "w_in": f(inputs["w_in"][0]), "w_out": f(inputs["w_out"][0]),
        "w_r": f(np.concatenate([np.asarray(inputs["w_group_router"][0]), np.asarray(inputs["w_expert_router"][0])], axis=1)),
        "b_r": f(np.concatenate([np.asarray(inputs["b_group_router"][0]), np.asarray(inputs["b_expert_router"][0])], axis=0)),
        "w_eg": f(inputs["w_exp_gate"][0]), "w_eu": f(inputs["w_exp_up"][0]), "w_ed": f(inputs["w_exp_down"][0]),
        "w_pp": f(inputs["w_ple_proj"][0]), "w_pg": f(inputs["w_ple_gate"][0]),
        "anw": f(np.asarray(inputs["attn_norm_w"][0]).reshape(8, 128).T), "sbw": f(inputs["sb_norm_w"][0]), "hlb": f(inputs["hg_lower_bounds"]),
        "hgw": f(inputs["hg_norm_w"][0]), "fnw": f(np.asarray(inputs["ffn_norm_w"][0]).reshape(8, 128).T), "plw": f(np.asarray(inputs["ple_norm_w"][0]).reshape(8, 128).T),
        "finw": f(inputs["final_norm_w"]), "fnwv": f(inputs["ffn_norm_w"][0]), "cb": cb, "cf": cf,
    }
    NT_ = S // 128
    NQ_ = 2 * NT_
    NSL_ = NQ_ + 64
    shared["pairid"] = (np.arange(NQ_, dtype=np.int32)[None, :] * 128 + np.arange(128, dtype=np.int32)[:, None]).astype(np.int32)
    li = np.broadcast_to((2 * S + np.arange(128, dtype=np.int32))[:, None], (128, NSL_)).astype(np.int32).copy()
    shared["linit"] = li
    shared["sstart"] = np.broadcast_to((np.arange(NSL_, dtype=np.float32) * 128.0)[None, :], (128, NSL_)).copy()
    shared["piota"] = np.arange(128, dtype=np.float32).reshape(128, 1)
    maps = []
    for b in range(n):
        m = dict(shared)
        m["x"] = f(np.asarray(inputs["x"])[b, :S])
        m["p"] = f(np.asarray(inputs["p"])[0, b, :S])
        maps.append(m)
    return maps


def kernel(**inputs):
    S = 4096
    nc = build(S)
    maps = make_in_maps(inputs, S, 8)
    res = run_bass_kernel_spmd(nc, maps, core_ids=list(range(8)))
    return np.stack([np.asarray(r["out"], dtype=np.float32) for r in res.results], axis=0)
```

```python
import numpy as np
import concourse.bass as bass
import concourse.mybir as mybir
from concourse.bass_utils import run_bass_kernel_spmd
from contextlib import ExitStack

F32 = mybir.dt.float32
BF16 = mybir.dt.bfloat16
I32 = mybir.dt.int32
AF = mybir.ActivationFunctionType
ALU = mybir.AluOpType
AX = mybir.AxisListType

COMPUTE = ("pe", "act", "dve", "pool")
ENGS = ("pe", "act", "dve", "pool", "sp")


class Buf:
    __slots__ = ("name", "lw", "rdc", "rdd")

    def __init__(self, name=""):
        self.name = name
        self.lw = None
        self.rdc = {}
        self.rdd = []


class Op:
    __slots__ = ("eng", "fn", "cdeps", "ddeps", "idx", "sig", "signo", "dma", "dsem", "dval", "gen")


class Prog:
    def __init__(self, n_dma_sems=20):
        self.ops = {e: [] for e in ENGS}
        self.n_dma_sems = n_dma_sems
        self.gen = 0
        self.barriers = []

    def barrier(self):
        self.barriers.append({e: len(self.ops[e]) for e in ENGS})
        self.gen += 1

    def add(self, eng, fn, reads=(), writes=(), dma=False):
        op = Op()
        op.eng = eng
        op.fn = fn
        op.dma = dma
        op.idx = len(self.ops[eng])
        op.cdeps = {}
        op.ddeps = []
        op.sig = False
        op.signo = 0
        op.dsem = None
        op.dval = 0
        op.gen = self.gen

        def dep(p):
            if p is None or p is op:
                return
            if p.dma:
                if p not in op.ddeps:
                    op.ddeps.append(p)
            else:
                if (not dma) and p.eng == "pe" and eng == "pe":
                    return
                cur = op.cdeps.get(p.eng)
                if cur is None or cur.idx < p.idx:
                    op.cdeps[p.eng] = p

        for b in reads:
            dep(b.lw)
        for b in writes:
            dep(b.lw)
            for r in b.rdc.values():
                dep(r)
            for r in b.rdd:
                dep(r)
        for b in reads:
            if dma:
                b.rdd.append(op)
            else:
                b.rdc[eng] = op
        for b in writes:
            b.lw = op
            b.rdc = {}
            b.rdd = []
        self.ops[eng].append(op)
        return op

    def pe(self, fn, reads=(), writes=()):
        return self.add("pe", fn, reads, writes)

    def act(self, fn, reads=(), writes=()):
        return self.add("act", fn, reads, writes)

    def dve(self, fn, reads=(), writes=()):
        return self.add("dve", fn, reads, writes)

    def pool(self, fn, reads=(), writes=()):
        return self.add("pool", fn, reads, writes)

    def dma(self, fn, reads=(), writes=(), q="sp"):
        return self.add(q, fn, reads, writes, dma=True)

    def emit(self, nc, stack):
        for e in ENGS:
            for op in self.ops[e]:
                for p in op.cdeps.values():
                    p.sig = True
        for snap in self.barriers:
            for e in COMPUTE:
                for op in reversed(self.ops[e][:snap[e]]):
                    if not op.dma:
                        op.sig = True
                        break
        for e in COMPUTE:
            n = 0
            for op in self.ops[e]:
                if op.sig and not op.dma:
                    n += 1
                    op.signo = n
        esem = {e: stack.enter_context(nc.semaphore("es_" + e)) for e in COMPUTE}
        dpool = {}
        for q in ("sp", "pool", "act"):
            if any(o.dma for o in self.ops[q]):
                dpool[q] = [stack.enter_context(nc.semaphore("ds_%s_%d" % (q, i)))
                            for i in range(self.n_dma_sems)]
        for q, sems in dpool.items():
            n = 0
            for op in self.ops[q]:
                if op.dma:
                    op.dsem = sems[n % len(sems)]
                    op.dval = 16 * (n // len(sems) + 1)
                    n += 1
        bar_waits = []
        for snap in self.barriers:
            wl = []
            for e in COMPUTE:
                for op in reversed(self.ops[e][:snap[e]]):
                    if not op.dma:
                        wl.append((esem[e], op.signo))
                        break
            for q in dpool:
                seen = {}
                for op in self.ops[q][:snap[q]]:
                    if op.dma:
                        seen[id(op.dsem)] = (op.dsem, op.dval)
                wl.extend(seen.values())
            bar_waits.append(wl)
        block = stack.enter_context(nc.Block())
        prog = self

        def run(ename, eng):
            waited = {}

            def wait(sem, val):
                k = id(sem)
                if waited.get(k, 0) >= val:
                    return
                eng.wait_ge(sem, val)
                waited[k] = val

            cur_gen = 0
            for op in prog.ops[ename]:
                while cur_gen < op.gen:
                    for sem, val in bar_waits[cur_gen]:
                        wait(sem, val)
                    cur_gen += 1
                for p in op.cdeps.values():
                    wait(esem[p.eng], p.signo)
                for p in op.ddeps:
                    wait(p.dsem, p.dval)
                if op.dma:
                    if op.dval > 16:
                        wait(op.dsem, op.dval - 16)
                    ins = op.fn(eng)
                    ins.then_inc(op.dsem, 16)
                else:
                    ins = op.fn(eng)
                    if op.sig:
                        ins.then_inc(esem[ename], 1)
            last = {}
            for op in prog.ops[ename]:
                if op.dma:
                    last[id(op.dsem)] = (op.dsem, op.dval)
            for sem, val in last.values():
                wait(sem, val)

        if self.ops["sp"]:
            @block.sync
            def _(e):
                run("sp", e)
        if self.ops["pe"]:
            @block.tensor
            def _(e):
                run("pe", e)
        if self.ops["act"]:
            @block.scalar
            def _(e):
                run("act", e)
        if self.ops["dve"]:
            @block.vector
            def _(e):
                run("dve", e)
        if self.ops["pool"]:
            @block.gpsimd
            def _(e):
                run("pool", e)

D = 1024
EPS = 1e-6
NEXP = 32
WB = 5
NCB = 128 * 5 + 4 * 512 + 8 + 1024 + 128
NCF = 256


class Arena:
    def __init__(self, nc, stack, n4):
        self.t = stack.enter_context(nc.sbuf_tensor("arena", [128, n4], F32))
        self.off = 0
        self.cap = n4

    def alloc(self, shape, dt):
        n = 1
        for s_ in shape[1:]:
            n *= s_
        nb = n * (2 if dt == BF16 else 4)
        n4 = (nb + 3) // 4
        n4 = (n4 + 7) // 8 * 8
        assert self.off + n4 <= self.cap, ("arena overflow", self.off, n4, self.cap)
        v = self.t[:, self.off:self.off + n4]
        self.off += n4
        if dt != F32:
            v = v.bitcast(dt)
        v = v[:, 0:n]
        if len(shape) == 3:
            v = v.rearrange("p (a b) -> p a b", a=shape[1])
        elif len(shape) == 4:
            v = v.rearrange("p (a b c) -> p a b c", a=shape[1], b=shape[2])
        return v


def run_interleaved(gens, W):
    pending = list(gens)
    active = [pending.pop(0) for _ in range(min(W, len(pending)))]
    while active:
        for g in list(active):
            try:
                next(g)
            except StopIteration:
                active.remove(g)
                if pending:
                    active.append(pending.pop(0))


class Rot:
    def __init__(self, items):
        self.items = list(items)
        self.i = 0

    def next(self):
        v = self.items[self.i % len(self.items)]
        self.i += 1
        return v


def build(S=4096, stop_after=None, debug_mix=False):
    nc = bass.Bass("TRN2", target_bir_lowering=False)
    NT = S // 128
    NS = S // 512
    PT = min(16, NT)
    NPASS = NT // PT

    def din(name, shape, dt=F32):
        return nc.dram_tensor(name, shape, dt, kind="ExternalInput").ap()

    x = din("x", [S, D])
    p_in = din("p", [S, 256])
    w_in = din("w_in", [D, 3584])
    w_out = din("w_out", [D, D])
    w_r = din("w_r", [D, 36])
    b_r = din("b_r", [36])
    w_eg = din("w_eg", [32, D, 512])
    w_eu = din("w_eu", [32, D, 512])
    w_ed = din("w_ed", [32, 512, D])
    w_pp = din("w_pp", [256, D])
    w_pg = din("w_pg", [D, D])
    anw = din("anw", [128, 8])
    sbw = din("sbw", [512])
    hlb = din("hlb", [2, 512])
    hgw = din("hgw", [512])
    fnw = din("fnw", [128, 8])
    plw = din("plw", [128, 8])
    finw = din("finw", [D])
    fnwv = din("fnwv", [D])
    cbd = din("cb", [128, NCB])
    cfd = din("cf", [128, NCF])
    out = nc.dram_tensor("out", [S, D], F32, kind="ExternalOutput").ap()
    pairid_d = nc.dram_tensor("pairid", [128, 2 * NT], I32, kind="ExternalInput").ap()
    linit_d = nc.dram_tensor("linit", [128, 2 * NT + 64], I32, kind="ExternalInput").ap()
    sstart_d = din("sstart", [128, 2 * NT + 64])
    piota_d = din("piota", [128, 1])
    m_scr = nc.dram_tensor("m_scr", [2 * S + 128, D], BF16, kind="Internal").ap()
    h1_scr = nc.dram_tensor("h1_scr", [S, D], F32, kind="Internal").ap()
    y_scr = nc.dram_tensor("y_scr", [2 * S + 128, D], F32, kind="Internal").ap()
    lst_i = nc.dram_tensor("lst_i", [(2 * NT + 64) * 128, 1], I32, kind="Internal").ap()
    lst_g = nc.dram_tensor("lst_g", [(2 * NT + 64) * 128, 1], F32, kind="Internal").ap()
    wgb = nc.dram_tensor("wgb", [32 * 128, 4096], BF16, kind="Internal").ap()
    wub = nc.dram_tensor("wub", [32 * 128, 4096], BF16, kind="Internal").ap()
    wdb = nc.dram_tensor("wdb", [32 * 128, 4096], BF16, kind="Internal").ap()
    if debug_mix:
        mix = nc.dram_tensor("mix_scr", [S, D], BF16, kind="ExternalOutput").ap()
    else:
        mix = nc.dram_tensor("mix_scr", [S, D], BF16, kind="Internal").ap()

    st = ExitStack()
    with st:
        P = Prog()
        AR = Arena(nc, st, 53000)
        pb = [st.enter_context(nc.psum_tensor("pb%d" % i, [128, 512], F32)) for i in range(8)]
        b_pb = [Buf("pb%d" % i) for i in range(8)]

        def mm(o, lhsT, rhs, start, stop, r, w):
            P.pe(lambda e, o=o, l=lhsT, rh=rhs, s0=start, s1=stop: e.matmul(o, lhsT=l, rhs=rh, start=s0, stop=s1), r, w)

        def tr(o, i, ident, r, w):
            P.pe(lambda e, o=o, i=i, d=ident: e.transpose(out=o, in_=i, identity=d), r, w)

        def actf(o, i, func, r, w, scale=1.0, bias=0.0, accum=None):
            if accum is None:
                P.act(lambda e, o=o, i=i, f=func, s=scale, b=bias: e.activation(out=o, in_=i, func=f, scale=s, bias=b), r, w)
            else:
                P.act(lambda e, o=o, i=i, f=func, s=scale, b=bias, a=accum: e.activation(out=o, in_=i, func=f, scale=s, bias=b, accum_out=a), r, w)

        def tt(eng, o, a, b, op, r, w):
            P.add(eng, lambda e, o=o, a=a, b=b, op=op: e.tensor_tensor(out=o, in0=a, in1=b, op=op), r, w)

        def ts(eng, o, a, s1, s2, op0, op1, r, w):
            if s2 is None:
                P.add(eng, lambda e, o=o, a=a, s1=s1, op0=op0: e.tensor_scalar(out=o, in0=a, scalar1=s1, scalar2=None, op0=op0), r, w)
            else:
                P.add(eng, lambda e, o=o, a=a, s1=s1, s2=s2, op0=op0, op1=op1: e.tensor_scalar(out=o, in0=a, scalar1=s1, scalar2=s2, op0=op0, op1=op1), r, w)

        def stt(eng, o, a, sc, b, op0, op1, r, w):
            P.add(eng, lambda e, o=o, a=a, sc=sc, b=b, op0=op0, op1=op1: e.scalar_tensor_tensor(out=o, in0=a, scalar=sc, in1=b, op0=op0, op1=op1), r, w)

        def cp(eng, o, i, r, w):
            if eng == "act":
                actf(o, i, AF.Copy, r, w)
            else:
                P.add(eng, lambda e, o=o, i=i: e.tensor_copy(out=o, in_=i), r, w)

        def recip(o, i, r, w):
            P.dve(lambda e, o=o, i=i: e.reciprocal(out=o, in_=i), r, w)

        def recip_act(o, i, r, w):
            actf(o, i, AF.Ln, r, w)
            actf(o, o, AF.Exp, w, w, scale=-1.0)

        def dma(o, i, r, w, q="sp"):
            P.dma(lambda e, o=o, i=i: e.dma_start(out=o, in_=i), r, w, q=q)

        def rstd_from_ss(ss_ap, n, r_buf):
            actf(ss_ap, ss_ap, AF.Ln, [r_buf], [r_buf], scale=1.0 / n, bias=EPS)
            actf(ss_ap, ss_ap, AF.Exp, [r_buf], [r_buf], scale=-0.5)

        def bf16view(bank):
            return bank[:, :].bitcast(BF16)

        cb = AR.alloc([128, NCB], BF16)
        b_cb = Buf("cb")
        cf = AR.alloc([128, NCF], F32)
        b_cf = Buf("cf")
        dma(cb, cbd, [], [b_cb], q="pool")
        dma(cf, cfd, [], [b_cb])
        idb = cb[:, 0:128]
        nuincl = cb[:, 128:256]
        nones = cb[:, 256:384]
        dmask = [cb[:, 640 + 512 * j: 640 + 512 * (j + 1)] for j in range(4)]
        pm0 = cb[:, 2688:2689]
        pm1 = cb[:, 2689:2690]
        cm0 = cb[:, 2696:2696 + 512]
        cm1 = cb[:, 2696 + 512:2696 + 1024]
        pones = cb[:, 3720:3848]
        trifwd = cb[:, 384:512]
        trirev = cb[:, 512:640]
        base_mark = AR.off

        aT = AR.alloc([128, 8, S], BF16)
        b_aT = [Buf("aT%d" % t) for t in range(NT)]
        nwA = AR.alloc([128, 8], F32)
        b_nwA = Buf()
        dma(nwA, anw, [], [b_nwA])
        sbw_b = AR.alloc([128, 512], F32)
        b_sbw = Buf()
        dma(sbw_b, sbw.partition_broadcast(128), [], [b_sbw])
        hgw_b = AR.alloc([128, 512], F32)
        b_hgw = Buf()
        dma(hgw_b, hgw.partition_broadcast(128), [], [b_hgw])
        lbraw = AR.alloc([128, 2, 512], F32)
        b_lbraw = Buf()
        dma(lbraw, hlb.partition_broadcast(128), [], [b_lbraw])
        oml_b = AR.alloc([128, 512], F32)
        b_oml = Buf()
        tt("dve", oml_b, lbraw[:, 1, :], lbraw[:, 0, :], ALU.subtract, [b_lbraw], [b_oml])
        actf(oml_b, oml_b, AF.Exp, [b_oml], [b_oml])
        ts("dve", lbraw[:, 0, :], oml_b, 1.0, None, ALU.add, None, [b_oml], [b_lbraw])
        recip(lbraw[:, 0, :], lbraw[:, 0, :], [b_lbraw], [b_lbraw])
        tt("dve", oml_b, oml_b, lbraw[:, 0, :], ALU.mult, [b_oml, b_lbraw], [b_oml])

        junk = AR.alloc([128, D], F32)
        b_junk = Buf()
        ssA = AR.alloc([128, NT], F32)
        b_ssA = [Buf() for _ in range(NT)]
        a1_mark = AR.off
        xs = [AR.alloc([128, D], F32) for _ in range(WB)]
        b_xs = [Buf() for _ in range(WB)]
        xn = [AR.alloc([128, D], BF16) for _ in range(WB)]
        b_xn = [Buf() for _ in range(WB)]
        psA = Rot([6, 7])
        def a1_tile(t):
            xt, bx = xs[t % WB], b_xs[t % WB]
            dma(xt, x[t * 128:(t + 1) * 128, :], [], [bx])
            sst = ssA[:, t:t + 1]
            actf(junk, xt, AF.Square, [bx], [b_junk, b_ssA[t]], accum=sst)
            rstd_from_ss(sst, D, b_ssA[t])
            yield
            xnt, bxn = xn[t % WB], b_xn[t % WB]
            ts("dve", xnt, xt, sst, None, ALU.mult, None, [bx, b_ssA[t]], [bxn])
            yield
            bi = psA.next()
            pv = bf16view(pb[bi]).rearrange("p (k n) -> p k n", k=8)
            for k in range(8):
                tr(pv[:, k, :], xnt[:, k * 128:(k + 1) * 128], idb, [bxn, b_cb], [b_pb[bi]])
            for k in range(8):
                ts("dve", aT[:, k, t * 128:(t + 1) * 128], pv[:, k, :], nwA[:, k:k + 1], None, ALU.mult, None,
                   [b_pb[bi], b_nwA], [b_aT[t]])

            yield
        run_interleaved([a1_tile(t) for t in range(NT)], WB)
        P.barrier()
        AR.off = a1_mark

        def aT_bufs(tt_):
            return [b_aT[4 * tt_ + j] for j in range(4)]

        qT = AR.alloc([128, S], BF16)
        b_qT = [Buf() for _ in range(NS)]
        kT = AR.alloc([128, S], BF16)
        b_kT = [Buf() for _ in range(NS)]
        Vp = AR.alloc([128, NT, 128], BF16)
        b_Vp = [Buf() for _ in range(NS)]
        wsl = [AR.alloc([128, 8, 128], BF16) for _ in range(8)]
        b_wsl = [Buf() for _ in range(8)]
        wrot = Rot(range(8))

        def load_w(col0):
            i = wrot.next()
            dma(wsl[i], w_in[:, col0:col0 + 128].rearrange("(k p) n -> p k n", p=128), [], [b_wsl[i]], q="pool")
            return i

        def proj_fm(wi, dst, dst_bufs, scale, evac_rot, prot):
            for g in range(NS):
                bi = prot.next()
                for k in range(8):
                    mm(pb[bi][:, :], wsl[wi][:, k, :], aT[:, k, g * 512:(g + 1) * 512], k == 0, k == 7,
                       aT_bufs(g) + [b_wsl[wi]], [b_pb[bi]])
                if evac_rot.next() == 0:
                    actf(dst[:, g * 512:(g + 1) * 512], pb[bi][:, :], AF.Copy, [b_pb[bi]], [dst_bufs[g]], scale=scale)
                else:
                    ts("dve", dst[:, g * 512:(g + 1) * 512], pb[bi][:, :], scale, None, ALU.mult, None, [b_pb[bi]], [dst_bufs[g]])

        def proj_tm_group(wi, g, bi):
            pvw = pb[bi][:, :].rearrange("p (j n) -> p j n", j=4)
            for j in range(4):
                t = 4 * g + j
                for k in range(8):
                    mm(pvw[:, j, :], aT[:, k, t * 128:(t + 1) * 128], wsl[wi][:, k, :], k == 0, k == 7,
                       [b_aT[t], b_wsl[wi]], [b_pb[bi]])
            return pvw

        sb_mark = AR.off
        NB = 3
        Eb = [AR.alloc([128, 512], F32) for _ in range(NB)]
        b_E = [Buf() for _ in range(NB)]
        Lpb = [AR.alloc([128, 512], BF16) for _ in range(NB)]
        b_Lp = [Buf() for _ in range(NB)]
        ATb = [AR.alloc([128, 512], BF16) for _ in range(NB)]
        b_AT = [Buf() for _ in range(NB)]
        Saccs = [AR.alloc([128, 512], BF16) for _ in range(2)]
        b_Sacc = [Buf() for _ in range(2)]
        qT_b = AR.alloc([128, S], BF16)
        kT_b = AR.alloc([128, S], BF16)
        Vp_b = AR.alloc([128, NT, 128], BF16)
        QTs, KTs, VPs = [qT, qT_b], [kT, kT_b], [Vp, Vp_b]
        B_QT = [b_qT, [Buf() for _ in range(NS)]]
        B_KT = [b_kT, [Buf() for _ in range(NS)]]
        B_VP = [b_Vp, [Buf() for _ in range(NS)]]

        def proj_pair_gen(pr_):
            c_ = pr_ % 2
            wq_i = load_w(pr_ * 128)
            wk_i = load_w(512 + pr_ * 128)
            wv_i = load_w(1024 + pr_ * 128)
            yield
            for wi_, dst_, bl_, sc_ in ((wq_i, QTs[c_], B_QT[c_], 0.125), (wk_i, KTs[c_], B_KT[c_], 1.0)):
                for g in range(NS):
                    for k in range(8):
                        mm(pb[7][:, :], wsl[wi_][:, k, :], aT[:, k, g * 512:(g + 1) * 512], k == 0, k == 7,
                           aT_bufs(g) + [b_wsl[wi_]], [b_pb[7]])
                    ts("dve", dst_[:, g * 512:(g + 1) * 512], pb[7][:, :], sc_, None, ALU.mult, None, [b_pb[7]], [bl_[g]])
                    yield
            for g in range(NS):
                pvw = proj_tm_group(wv_i, g, 7)
                cp("dve", VPs[c_][:, 4 * g:4 * g + 4, :], pvw, [b_pb[7]], [B_VP[c_][g]])
                yield

        osb = [AR.alloc([128, 4, 64], F32) for _ in range(2)]
        b_osb = [Buf() for _ in range(2)]
        osq = AR.alloc([128, 4, 64], F32)
        b_osq = Buf()
        ssn = [AR.alloc([128, 4], F32) for _ in range(2)]
        b_ssn = [Buf() for _ in range(2)]
        mixo = [AR.alloc([128, 4, 64], BF16) for _ in range(2)]
        b_mixo = [Buf() for _ in range(2)]
        evr = Rot([0, 1])
        gi_ctr = [0]
        b_wcast = Buf("wcast")
        cast_jobs = []
        for e_i in range(32):
            cast_jobs.append((wgb[e_i * 128:(e_i + 1) * 128, :].rearrange("p (k n) -> p k n", k=8), w_eg[e_i].rearrange("(k p) n -> p k n", p=128)))
            cast_jobs.append((wub[e_i * 128:(e_i + 1) * 128, :].rearrange("p (k n) -> p k n", k=8), w_eu[e_i].rearrange("(k p) n -> p k n", p=128)))
            cast_jobs.append((wdb[e_i * 128:(e_i + 1) * 128, :].rearrange("p (k n) -> p k n", k=4), w_ed[e_i].rearrange("(k p) n -> p k n", p=128)))
        cast_every = max(1, (4 * 2 * (NS * (NS + 1) * 2)) // 100)
        cast_ctr = [0]

        def maybe_cast():
            cast_ctr[0] += 1
            if cast_ctr[0] % cast_every == 0 and cast_jobs:
                o_, i_ = cast_jobs.pop(0)
                dma(o_, i_, [], [], q="pool")
        for _ in proj_pair_gen(0):
            pass
        for pr in range(4):
            qT, kT, Vp = QTs[pr % 2], KTs[pr % 2], VPs[pr % 2]
            b_qT, b_kT, b_Vp = B_QT[pr % 2], B_KT[pr % 2], B_VP[pr % 2]
            nxt_proj = proj_pair_gen(pr + 1) if pr < 3 else None
            units = []
            for hh in range(2):
                for i in range(NS):
                    kbs = list(range(4 * i + 3, -1, -1))
                    for kb in kbs:
                        units.append(dict(hb=hh * 64, head=pr * 2 + hh, i=i, kb=kb, j=kb - 4 * i,
                                          first=(kb == kbs[0]), last=(kb == 0), gi=None))
            gcount = gi_ctr[0]
            for u_ in units:
                if u_["first"]:
                    gcount += 1
                u_["gi"] = gcount
            gi_ctr[0] = gcount
            n = len(units)
            sacc_cur = [0]

            def st0(ix):
                u_ = units[ix]
                hb, i, kb, j = u_["hb"], u_["i"], u_["kb"], u_["j"]
                zi = ix % 2
                diag = j >= 0
                c0 = 128 * max(j, 0)
                mm(pb[zi][:, c0:], kT[hb:hb + 64, kb * 128:(kb + 1) * 128], qT[hb:hb + 64, i * 512 + c0:(i + 1) * 512],
                   True, not diag, [b_kT[kb // 4], b_qT[i]], [b_pb[zi]])
                if diag:
                    mm(pb[zi][:, c0:], idb, dmask[j][:, c0:], False, True, [b_cb], [b_pb[zi]])

            def st1a(ix):
                zi = ix % 2
                c0 = 128 * max(units[ix]["j"], 0)
                actf(Eb[ix % NB][:, c0:], pb[zi][:, c0:], AF.Exp, [b_pb[zi]], [b_E[ix % NB]])

            def st1b(ix):
                c0 = 128 * max(units[ix]["j"], 0)
                actf(Lpb[ix % NB][:, c0:], Eb[ix % NB][:, c0:], AF.Ln, [b_E[ix % NB]], [b_Lp[ix % NB]], bias=1.0)

            def st2(ix):
                u_ = units[ix]
                hb, i, kb, j = u_["hb"], u_["i"], u_["kb"], u_["j"]
                ci = 2 + (ix % 3)
                diag = j >= 0
                lp, blp = Lpb[ix % NB], b_Lp[ix % NB]
                cur = sacc_cur[0]
                c0 = 128 * max(j, 0)
                mm(pb[ci][:, c0:], kT[hb:hb + 64, kb * 128:(kb + 1) * 128], qT[hb:hb + 64, i * 512 + c0:(i + 1) * 512],
                   True, False, [b_kT[kb // 4], b_qT[i]], [b_pb[ci]])
                mm(pb[ci][:, c0:], nuincl, lp[:, c0:], False, (u_["first"] and not diag), [b_cb, blp], [b_pb[ci]])
                if not u_["first"]:
                    mm(pb[ci][:, c0:], nones, Saccs[cur][:, c0:], False, not diag, [b_cb, b_Sacc[cur]], [b_pb[ci]])
                if diag:
                    mm(pb[ci][:, c0:], idb, dmask[j][:, c0:], False, True, [b_cb], [b_pb[ci]])
                if not u_["last"]:
                    nxt = 1 - cur
                    if u_["first"]:
                        P.pool(lambda e, o=Saccs[nxt][:, 0:384]: e.memset(o, 0.0), [], [b_Sacc[nxt]])
                        P.pool(lambda e, o=Saccs[cur][:, 0:256]: e.memset(o, 0.0), [], [b_Sacc[cur]])
                        cp("pool", Saccs[nxt][:, c0:], lp[:, c0:], [blp], [b_Sacc[nxt]])
                    else:
                        tt("pool", Saccs[nxt][:, c0:], Saccs[cur][:, c0:], lp[:, c0:], ALU.add, [b_Sacc[cur], blp], [b_Sacc[nxt]])
                    sacc_cur[0] = nxt

            def st3(ix):
                ci = 2 + (ix % 3)
                c0 = 128 * max(units[ix]["j"], 0)
                actf(ATb[ix % NB][:, c0:], pb[ci][:, c0:], AF.Exp, [b_pb[ci]], [b_AT[ix % NB]])

            def st4(ix):
                u_ = units[ix]
                hb, i, kb, j, head = u_["hb"], u_["i"], u_["kb"], u_["j"], u_["head"]
                obi = 5 + (u_["gi"] % 2)
                Ov = pb[obi][:, 0:256].rearrange("p (s c) -> p s c", s=4)
                at, bat = ATb[ix % NB], b_AT[ix % NB]
                for sub in range(4):
                    if j >= 0 and sub < j:
                        continue
                    P.pe(lambda e, o=Ov[:, sub, :], l=at[:, sub * 128:(sub + 1) * 128], rh=Vp[:, kb, hb:hb + 64],
                         s0=(u_["first"] and sub == 3), s1=(kb == 0 and sub == 3):
                         e.matmul(o, lhsT=l, rhs=rh, start=s0, stop=s1, skip_group_check=True),
                         [bat, b_Vp[kb // 4]], [b_pb[obi]])
                if u_["last"]:
                    gi = u_["gi"]
                    o_, bo = osb[gi % 2], b_osb[gi % 2]
                    sn, bsn = ssn[gi % 2], b_ssn[gi % 2]
                    mo, bmo = mixo[gi % 2], b_mixo[gi % 2]
                    cp("dve", o_, Ov, [b_pb[obi]], [bo])
                    tt("pool", osq, o_, o_, ALU.mult, [bo], [b_osq])
                    P.dve(lambda e, o=sn, i_=osq: e.tensor_reduce(out=o, in_=i_, axis=AX.X, op=ALU.add), [b_osq], [bsn])
                    rstd_from_ss(sn, 64, bsn)
                    tt("dve", o_, o_, sn.unsqueeze(2).to_broadcast([128, 4, 64]), ALU.mult, [bo, bsn], [bo])
                    tt("dve", mo, o_, sbw_b[:, head * 64:(head + 1) * 64].unsqueeze(1).to_broadcast([128, 4, 64]),
                       ALU.mult, [bo, b_sbw], [bmo])
                    dma(mix[i * 512:(i + 1) * 512, head * 64:(head + 1) * 64].rearrange("(s p) c -> p s c", p=128),
                        mo, [bmo], [])

            pf_every = max(1, (n + 3) // (3 * NS + 4))
            for s_ in range(n + 3):
                maybe_cast()
                if nxt_proj is not None and s_ % pf_every == 0:
                    next(nxt_proj, None)
                if s_ < n:
                    st0(s_)
                if 1 <= s_ <= n:
                    st1a(s_ - 1)
                if 3 <= s_ <= n + 2:
                    st3(s_ - 3)
                if 1 <= s_ <= n:
                    st1b(s_ - 1)
                    st2(s_ - 1)
                if 3 <= s_ <= n + 2:
                    st4(s_ - 3)
            if nxt_proj is not None:
                for _ in nxt_proj:
                    pass

        while cast_jobs:
            o_c, i_c = cast_jobs.pop(0)
            dma(o_c, i_c, [], [], q="pool")
        if stop_after == "sb":
            P.emit(nc, st)
            return nc

        qT, kT, Vp = QTs[0], KTs[0], VPs[0]
        b_qT, b_kT, b_Vp = B_QT[0], B_KT[0], B_VP[0]
        P.barrier()
        AR.off = sb_mark
        import os as _os
        _hgstop = int(_os.environ.get("HG_STOP", "0"))

        class _Stop(Exception):
            pass

        def chk(n):
            if _hgstop == n:
                raise _Stop()
        def HG_BODY():
            nonlocal wrot
            eb = AR.alloc([128, S], F32)
            b_eb = [Buf() for _ in range(NS)]
            ktok2 = AR.alloc([128, NT, 128], BF16)
            ktok2h = AR.alloc([128, NT, 128], BF16)
            qTh = AR.alloc([128, S], BF16)
            print('arena after HG big allocs', AR.off, AR.cap)
            b_k2 = [Buf() for _ in range(NS)]
            gw = AR.alloc([128, NT, 128], BF16)
            b_gw = [Buf() for _ in range(NS)]
            t1s = [AR.alloc([128, 512], F32) for _ in range(2)]
            b_t1 = [Buf() for _ in range(2)]
            t2s = [AR.alloc([128, 512], F32) for _ in range(2)]
            b_t2 = [Buf() for _ in range(2)]
            ktk = [AR.alloc([128, 4, 128], F32) for _ in range(2)]
            b_ktk = [Buf() for _ in range(2)]
            ktkb = [AR.alloc([128, 4, 128], BF16) for _ in range(2)]
            b_ktkb = [Buf() for _ in range(2)]
            gtk = [AR.alloc([128, 4, 128], F32) for _ in range(2)]
            b_gtk = [Buf() for _ in range(2)]
            ghis = [AR.alloc([128, 4, 128], BF16) for _ in range(2)]
            glos = [AR.alloc([128, 4, 128], BF16) for _ in range(2)]
            b_gh = [Buf() for _ in range(2)]
            enb = [AR.alloc([128, 512], F32) for _ in range(2)]
            b_enb = [Buf() for _ in range(2)]
            Sst = [AR.alloc([128, 128], F32) for _ in range(4)]
            b_Sst = [Buf() for _ in range(4)]
            Sbf = [AR.alloc([128, 128], BF16) for _ in range(4)]
            b_Sbf = [Buf() for _ in range(4)]
            smb = [AR.alloc([128, 128], BF16) for _ in range(2)]
            b_smb = [Buf() for _ in range(2)]
            ssh = [AR.alloc([128, 1], F32) for _ in range(4)]
            b_ssh = [Buf() for _ in range(4)]
            mixh = [AR.alloc([128, 4, 128], BF16) for _ in range(2)]
            b_mixh = [Buf() for _ in range(2)]
            protP = Rot([6, 7])
            protR = Rot([0, 1, 2, 3, 4, 5])
            wts = {}
            scs = [0]

            def proj_gen(hh, g, tix):
                if g == 0:
                    wts[hh] = (load_w(1536 + hh * 128), load_w(2048 + hh * 128), load_w(2560 + hh * 128), load_w(3072 + hh * 128))
                wq_i, wf_i, wi_i, wg_i = wts[hh]
                hs = slice(hh * 128, (hh + 1) * 128)
                gs = slice(g * 512, (g + 1) * 512)
                t1, bt1 = t1s[tix % 2], b_t1[tix % 2]
                t2, bt2 = t2s[tix % 2], b_t2[tix % 2]
                kt, bkt = ktk[tix % 2], b_ktk[tix % 2]
                ktb, bktb = ktkb[tix % 2], b_ktkb[tix % 2]
                gt, bgt = gtk[tix % 2], b_gtk[tix % 2]
                en, ben = enb[tix % 2], b_enb[tix % 2]
                ghi, glo = ghis[tix % 2], glos[tix % 2]
                bgh = b_gh[tix % 2]
                t1v = t1.rearrange("p (j n) -> p j n", j=4)
                t2v = t2.rearrange("p (j n) -> p j n", j=4)
                bi = protP.next()
                pf = proj_tm_group(wf_i, g, bi)
                actf(t1v, pf, AF.Exp, [b_pb[bi]], [bt1], scale=-1.0)
                yield
                ts("dve", t2, t1, 1.0, None, ALU.add, None, [bt1], [bt2])
                recip_act(t2, t2, [bt2], [bt2])
                tt("dve", t1, t1, t2, ALU.mult, [bt1, bt2], [bt1])
                tt("dve", kt, t1v, oml_b[:, hs].unsqueeze(1).to_broadcast([128, 4, 128]), ALU.mult,
                   [bt1, b_oml], [bkt])
                yield
                actf(gt, kt, AF.Ln, [bkt], [bgt], scale=-1.0, bias=1.0)
                cp("pool", ktb, kt, [bkt], [bktb])
                cp("pool", ghi, gt, [bgt], [bgh])
                tt("dve", glo, gt, ghi, ALU.subtract, [bgt, bgh], [bgh])
                yield
                bi = protP.next()
                pc = pb[bi][:, :].rearrange("p (j n) -> p j n", j=4)
                for j in range(4):
                    mm(pc[:, j, :], ghi[:, j, :], trifwd, True, False, [bgh, b_cb], [b_pb[bi]])
                    mm(pc[:, j, :], glo[:, j, :], trifwd, False, True, [bgh, b_cb], [b_pb[bi]])
                actf(eb[:, gs], pb[bi][:, :], AF.Exp, [b_pb[bi]], [b_eb[g]])
                actf(en, pb[bi][:, :], AF.Exp, [b_pb[bi]], [ben], scale=-1.0)
                yield
                bi = protP.next()
                prv = pb[bi][:, :].rearrange("p (j n) -> p j n", j=4)
                for j in range(4):
                    mm(prv[:, j, :], trirev, ghi[:, j, :], True, False, [bgh, b_cb], [b_pb[bi]])
                    mm(prv[:, j, :], trirev, glo[:, j, :], False, True, [bgh, b_cb], [b_pb[bi]])
                actf(t2, pb[bi][:, :], AF.Exp, [b_pb[bi]], [bt2])
                yield
                stt("dve", ktok2[:, 4 * g:4 * g + 4, :], kt, pm0, t2v, ALU.mult, ALU.mult, [bkt, bt2, b_cb], [b_k2[g]])
                stt("dve", ktok2h[:, 4 * g:4 * g + 4, :], kt, pm1, t2v, ALU.mult, ALU.mult, [bkt, bt2, b_cb], [b_k2[g]])
                yield
                bi = protP.next()
                pk = bf16view(pb[bi])[:, 0:512].rearrange("p (j n) -> p j n", j=4)
                for j in range(4):
                    tr(pk[:, j, :], ktb[:, j, :], idb, [bktb, b_cb], [b_pb[bi]])
                tt("dve", kT[:, gs], bf16view(pb[bi])[:, 0:512], en, ALU.mult, [b_pb[bi], ben], [b_kT[g]])
                yield
                bi = protP.next()
                for k in range(8):
                    mm(pb[bi][:, :], wsl[wq_i][:, k, :], aT[:, k, gs], k == 0, k == 7, aT_bufs(g) + [b_wsl[wq_i]], [b_pb[bi]])
                actf(t1, pb[bi][:, :], AF.Exp, [b_pb[bi]], [bt1], scale=-1.0)
                ts("dve", t1, t1, 1.0, None, ALU.add, None, [bt1], [bt1])
                recip_act(t1, t1, [bt1], [bt1])
                tt("dve", t1, pb[bi][:, :], t1, ALU.mult, [b_pb[bi], bt1], [bt1])
                yield
                tt("dve", t1, t1, eb[:, gs], ALU.mult, [bt1, b_eb[g]], [bt1])
                tt("dve", qT[:, gs], t1, cm0, ALU.mult, [bt1, b_cb], [b_qT[g]])
                tt("dve", qTh[:, gs], t1, cm1, ALU.mult, [bt1, b_cb], [b_qT[g]])
                yield
                bi = protP.next()
                pvw = proj_tm_group(wi_i, g, bi)
                cp("act", Vp[:, 4 * g:4 * g + 4, :], pvw, [b_pb[bi]], [b_Vp[g]])
                yield
                bi = protP.next()
                pg = proj_tm_group(wg_i, g, bi)
                actf(t2v, pg, AF.Exp, [b_pb[bi]], [bt2], scale=-1.0)
                ts("dve", t2, t2, 1.0, None, ALU.add, None, [bt2], [bt2])
                recip_act(t2, t2, [bt2], [bt2])
                tt("dve", t2v, pg, t2v, ALU.mult, [b_pb[bi], bt2], [bt2])
                yield
                tt("dve", gw[:, 4 * g:4 * g + 4, :], t2v, hgw_b[:, hs].unsqueeze(1).to_broadcast([128, 4, 128]), ALU.mult,
                   [bt2, b_hgw], [b_gw[g]])
                yield

            def rec_gen(hh, g):
                if g == 0:
                    scs[0] = 0
                    P.dve(lambda e, o=Sst[0]: e.memset(o, 0.0), [], [b_Sst[0]])
                    P.dve(lambda e, o=Sbf[0]: e.memset(o, 0.0), [], [b_Sbf[0]])
                for t in range(4 * g, 4 * g + 4):
                    sc = scs[0]
                    tsl = slice(t * 128, (t + 1) * 128)
                    bs = protR.next()
                    mm(pb[bs][:, 0:128], kT[:, tsl], qT[:, tsl], True, False, [b_kT[g], b_qT[g]], [b_pb[bs]])
                    mm(pb[bs][:, 0:128], kT[:, tsl], qTh[:, tsl], False, True, [b_kT[g], b_qT[g]], [b_pb[bs]])
                    bu = protR.next()
                    Uv = pb[bu][:, 0:256].rearrange("p (j n) -> p j n", j=2)
                    mm(Uv[:, 0, :], ktok2[:, t, :], Vp[:, t, :], True, True, [b_k2[g], b_Vp[g]], [b_pb[bu]])
                    mm(Uv[:, 1, :], ktok2h[:, t, :], Vp[:, t, :], True, True, [b_k2[g], b_Vp[g]], [b_pb[bu]])
                    sm, bsm = smb[t % 2], b_smb[t % 2]
                    tt("dve", sm, pb[bs][:, 0:128], trifwd, ALU.mult, [b_pb[bs], b_cb], [bsm])
                    s0, s1, s2 = sc % 4, (sc + 1) % 4, (sc + 2) % 4
                    c0 = 2 * t
                    stt("dve", Sst[s1], Sst[s0], eb[:, c0 * 64 + 63:c0 * 64 + 64], Uv[:, 0, :], ALU.mult, ALU.add,
                        [b_Sst[s0], b_eb[g], b_pb[bu]], [b_Sst[s1]])
                    cp("pool", Sbf[s1], Sst[s1], [b_Sst[s1]], [b_Sbf[s1]])
                    c1 = 2 * t + 1
                    stt("dve", Sst[s2], Sst[s1], eb[:, c1 * 64 + 63:c1 * 64 + 64], Uv[:, 1, :], ALU.mult, ALU.add,
                        [b_Sst[s1], b_eb[g], b_pb[bu]], [b_Sst[s2]])
                    cp("pool", Sbf[s2], Sst[s2], [b_Sst[s2]], [b_Sbf[s2]])
                    scs[0] = sc + 2
                    yield
                    bo_ = protR.next()
                    Op_ = pb[bo_][:, 0:128]
                    mm(Op_, sm, Vp[:, t, :], True, False, [bsm, b_Vp[g]], [b_pb[bo_]])
                    mm(Op_, qT[:, tsl], Sbf[s0], False, False, [b_qT[g], b_Sbf[s0]], [b_pb[bo_]])
                    mm(Op_, qTh[:, tsl], Sbf[s1], False, True, [b_qT[g], b_Sbf[s1]], [b_pb[bo_]])
                    sh, bsh = ssh[t % 4], b_ssh[t % 4]
                    actf(junk[:, 0:128], Op_, AF.Square, [b_pb[bo_]], [b_junk, bsh], accum=sh)
                    rstd_from_ss(sh, 128, bsh)
                    yield
                    mh, bmh = mixh[(t // 4) % 2], b_mixh[(t // 4) % 2]
                    stt("dve", mh[:, t % 4, :], Op_, sh, gw[:, t, :], ALU.mult, ALU.mult, [b_pb[bo_], bsh, b_gw[g]], [bmh])
                    if t % 4 == 3:
                        dma(mix[(t - 3) * 128:(t + 1) * 128, 512 + hh * 128:512 + (hh + 1) * 128].rearrange("(j p) c -> p j c", p=128),
                            mh, [bmh], [])
                    yield

            stages = [(hh, g) for hh in range(4) for g in range(NS)]
            prev = None
            for k_, (hh, g) in enumerate(stages):
                gens = [proj_gen(hh, g, k_)]
                if prev is not None:
                    gens.append(prev)
                run_interleaved(gens, 2)
                prev = rec_gen(hh, g)
            run_interleaved([prev], 1)

        try:
            HG_BODY()
        except _Stop:
            P.emit(nc, st)
            return nc
        if stop_after == "hg":
            P.emit(nc, st)
            return nc
        P.barrier()
        AR.off = base_mark
        NQ = 2 * NT
        NSLOT = NQ + 64
        RROWS = NSLOT * 128
        OOBV = 1 << 20
        b_msc = Buf("m_scr")
        b_h1sc = Buf("h1_scr")
        b_ysc = Buf("y_scr")
        b_lst = Buf("lst")
        finw_b = AR.alloc([128, D], F32)
        b_finw = Buf()
        dma(finw_b, finw.partition_broadcast(128), [], [b_finw])
        plwT = AR.alloc([128, 8], F32)
        b_nw2 = Buf()
        dma(plwT, plw, [], [b_nw2])
        junkB = AR.alloc([128, D], F32)
        b_junkB = Buf()
        OHall = AR.alloc([128, NQ, 32], BF16)
        b_OH = [Buf() for _ in range(NQ)]
        gall = AR.alloc([128, NQ], F32)
        b_gall = [Buf() for _ in range(NQ)]
        pass_mark = AR.off
        prot = Rot(range(8))
        fnw_b = AR.alloc([128, D], F32)
        b_fnwb = Buf()
        dma(fnw_b, fnwv.partition_broadcast(128), [], [b_fnwb])
        br_b = AR.alloc([128, 36], F32)
        b_br = Buf()
        dma(br_b, b_r.partition_broadcast(128), [], [b_br])
        wr32 = AR.alloc([128, 8, 36], F32)
        b_wr = Buf()
        dma(wr32, w_r.rearrange("(k p) n -> p k n", p=128), [], [b_wr])
        wrhi = AR.alloc([128, 8, 36], BF16)
        wrlo = AR.alloc([128, 8, 36], BF16)
        cp("dve", wrhi, wr32, [b_wr], [b_wr])
        tt("dve", wrlo, wr32, wrhi, ALU.subtract, [b_wr], [b_wr])
        wo = AR.alloc([128, 8, D], BF16)
        b_wo = Buf()
        dma(wo, w_out.rearrange("(k p) n -> p k n", p=128), [], [b_wo], q="pool")
        xts = [AR.alloc([128, D], F32) for _ in range(WB)]
        b_xts = [Buf() for _ in range(WB)]
        mts = [AR.alloc([128, D], BF16) for _ in range(WB)]
        b_mts = [Buf() for _ in range(WB)]
        mxT = [AR.alloc([128, 8, 128], BF16) for _ in range(WB)]
        b_mxT = [Buf() for _ in range(WB)]
        h1t = [AR.alloc([128, D], F32) for _ in range(WB)]
        b_h1t = [Buf() for _ in range(WB)]
        mn32 = [AR.alloc([128, D], F32) for _ in range(WB)]
        b_mn32 = [Buf() for _ in range(WB)]
        mhi = [AR.alloc([128, D], BF16) for _ in range(WB)]
        b_mhi = [Buf() for _ in range(WB)]
        mlo = [AR.alloc([128, D], BF16) for _ in range(WB)]
        b_mlo = [Buf() for _ in range(WB)]
        mhiT = [AR.alloc([128, 8, 128], BF16) for _ in range(WB)]
        b_mhiT = [Buf() for _ in range(WB)]
        mloT = [AR.alloc([128, 8, 128], BF16) for _ in range(WB)]
        b_mloT = [Buf() for _ in range(WB)]
        rt = [AR.alloc([128, 160], F32) for _ in range(WB)]
        b_rt = [Buf() for _ in range(WB)]
        def b1_tile(t):
            s_ = t % WB
            xt, bx = xts[s_], b_xts[s_]
            mt, bm = mts[s_], b_mts[s_]
            dma(xt, x[t * 128:(t + 1) * 128, :], [], [bx])
            dma(mt, mix[t * 128:(t + 1) * 128, :], [], [bm])
            bi = prot.next()
            pv = bf16view(pb[bi]).rearrange("p (k n) -> p k n", k=8)
            for k in range(8):
                tr(pv[:, k, :], mt[:, k * 128:(k + 1) * 128], idb, [bm, b_cb], [b_pb[bi]])
            cp("act", mxT[s_], pv, [b_pb[bi]], [b_mxT[s_]])
            yield
            h1, bh1 = h1t[s_], b_h1t[s_]
            for half in range(2):
                hsl = slice(half * 512, (half + 1) * 512)
                bi = prot.next()
                for k in range(8):
                    mm(pb[bi][:, :], mxT[s_][:, k, :], wo[:, k, hsl], k == 0, k == 7, [b_mxT[s_], b_wo], [b_pb[bi]])
                tt("dve", h1[:, hsl], pb[bi][:, :], xt[:, hsl], ALU.add, [b_pb[bi], bx], [bh1])
            dma(h1_scr[t * 128:(t + 1) * 128, :], h1, [bh1], [])
            r_, br_ = rt[s_], b_rt[s_]
            yield
            ss1 = r_[:, 0:1]
            actf(junkB, h1, AF.Square, [bh1], [b_junkB, br_], accum=ss1)
            rstd_from_ss(ss1, D, br_)
            yield
            stt("dve", mn32[s_], h1, ss1, fnw_b, ALU.mult, ALU.mult, [bh1, br_, b_fnwb], [b_mn32[s_]])
            yield
            cp("act", mhi[s_], mn32[s_], [b_mn32[s_]], [b_mhi[s_]])
            yield
            tt("dve", mlo[s_], mn32[s_], mhi[s_], ALU.subtract, [b_mn32[s_], b_mhi[s_]], [b_mlo[s_]])
            yield
            dma(m_scr[t * 128:(t + 1) * 128, :], mhi[s_], [b_mhi[s_]], [])
            dma(m_scr[S + t * 128:S + (t + 1) * 128, :], mhi[s_], [b_mhi[s_]], [])
            bi = prot.next()
            pvh = bf16view(pb[bi]).rearrange("p (k n) -> p k n", k=8)
            for k in range(8):
                tr(pvh[:, k, :], mhi[s_][:, k * 128:(k + 1) * 128], idb, [b_mhi[s_], b_cb], [b_pb[bi]])
            cp("act", mhiT[s_], pvh, [b_pb[bi]], [b_mhiT[s_]])
            yield
            bi = prot.next()
            pvl = bf16view(pb[bi]).rearrange("p (k n) -> p k n", k=8)
            for k in range(8):
                tr(pvl[:, k, :], mlo[s_][:, k * 128:(k + 1) * 128], idb, [b_mlo[s_], b_cb], [b_pb[bi]])
            cp("dve", mloT[s_], pvl, [b_pb[bi]], [b_mloT[s_]])
            yield
            bi = prot.next()
            for k in range(8):
                mm(pb[bi][:, 0:36], mhiT[s_][:, k, :], wrhi[:, k, :], k == 0, False, [b_mhiT[s_], b_wr], [b_pb[bi]])
                mm(pb[bi][:, 0:36], mloT[s_][:, k, :], wrhi[:, k, :], False, False, [b_mloT[s_], b_wr], [b_pb[bi]])
                mm(pb[bi][:, 0:36], mhiT[s_][:, k, :], wrlo[:, k, :], False, k == 7, [b_mhiT[s_], b_wr], [b_pb[bi]])
            lg = r_[:, 8:44]
            gmax = r_[:, 1:2]
            ngmax = r_[:, 2:3]
            sg_ = r_[:, 3:4]
            gprob = r_[:, 4:5]
            v1 = r_[:, 5:6]
            v2 = r_[:, 6:7]
            dd = r_[:, 7:8]
            oh = r_[:, 44:48]
            pen = r_[:, 48:52]
            egj = r_[:, 52:56]
            elm = r_[:, 56:88]
            is1 = r_[:, 88:120]
            is2 = r_[:, 120:152]
            e2 = r_[:, 152:153]
            p1 = r_[:, 153:154]
            p2 = r_[:, 154:155]
            R = [br_]
            tt("dve", lg, pb[bi][:, 0:36], br_b, ALU.add, [b_pb[bi], b_br], R)
            yield
            P.dve(lambda e, o=gmax, i_=lg[:, 0:4]: e.tensor_reduce(out=o, in_=i_, axis=AX.X, op=ALU.max), R, R)
            ts("dve", oh, lg[:, 0:4], gmax, None, ALU.is_equal, None, R, R)
            ts("dve", ngmax, gmax, -1.0, None, ALU.mult, None, R, R)
            actf(egj, lg[:, 0:4], AF.Exp, R, R, bias=ngmax, accum=sg_)
            yield
            recip(gprob, sg_, R, R)
            ts("dve", pen, oh, 1.0, 1e30, ALU.subtract, ALU.mult, R, R)
            tt("dve", elm.rearrange("p (g e) -> p g e", g=4), lg[:, 4:36].rearrange("p (g e) -> p g e", g=4),
               pen.unsqueeze(2).to_broadcast([128, 4, 8]), ALU.add, R, R)
            P.dve(lambda e, o=v1, i_=elm: e.tensor_reduce(out=o, in_=i_, axis=AX.X, op=ALU.max), R, R)
            ts("dve", is1, elm, v1, None, ALU.is_equal, None, R, R)
            stt("dve", elm, is1, -1e30, elm, ALU.mult, ALU.add, R, R)
            P.dve(lambda e, o=v2, i_=elm: e.tensor_reduce(out=o, in_=i_, axis=AX.X, op=ALU.max), R, R)
            ts("dve", is2, elm, v2, None, ALU.is_equal, None, R, R)
            tt("dve", dd, v2, v1, ALU.subtract, R, R)
            actf(e2, dd, AF.Exp, R, R)
            yield
            ts("dve", p1, e2, 1.0, None, ALU.add, None, R, R)
            recip(p1, p1, R, R)
            tt("dve", p2, e2, p1, ALU.mult, R, R)
            tt("dve", gall[:, t:t + 1], p1, gprob, ALU.mult, R, [b_gall[t]])
            tt("dve", gall[:, NT + t:NT + t + 1], p2, gprob, ALU.mult, R, [b_gall[NT + t]])
            cp("dve", OHall[:, t, :], is1, R, [b_OH[t]])
            cp("dve", OHall[:, NT + t, :], is2, R, [b_OH[NT + t]])
        run_interleaved([b1_tile(t) for t in range(NT)], WB)
        if stop_after == "b1":
            P.emit(nc, st)
            return nc
        P.barrier()
        AR.off = pass_mark
        Cacc = [AR.alloc([128, 32], BF16) for _ in range(2)]
        b_Cacc = [Buf() for _ in range(2)]
        rk = AR.alloc([128, NQ], F32)
        b_rk = Buf()
        tmp32 = [AR.alloc([128, 32], F32) for _ in range(2)]
        b_tmp32 = [Buf() for _ in range(2)]
        P.dve(lambda e, o=Cacc[0]: e.memset(o, 0.0), [], [b_Cacc[0]])
        for q in range(NQ):
            bi = prot.next()
            cc = q % 2
            mm(pb[bi][:, 0:32], pones, OHall[:, q, :], True, False, [b_cb, b_OH[q]], [b_pb[bi]])
            mm(pb[bi][:, 0:32], nuincl, OHall[:, q, :], False, q == 0, [b_cb, b_OH[q]], [b_pb[bi]])
            if q > 0:
                mm(pb[bi][:, 0:32], pones, Cacc[cc], False, True, [b_cb, b_Cacc[cc]], [b_pb[bi]])
            tt("dve", tmp32[cc], pb[bi][:, 0:32], OHall[:, q, :], ALU.mult, [b_pb[bi], b_OH[q]], [b_tmp32[cc]])
            P.dve(lambda e, o=rk[:, q:q + 1], i_=tmp32[cc]: e.tensor_reduce(out=o, in_=i_, axis=AX.X, op=ALU.add), [b_tmp32[cc]], [b_rk])
            tt("pool", Cacc[1 - cc], Cacc[cc], OHall[:, q, :], ALU.add, [b_Cacc[cc], b_OH[q]], [b_Cacc[1 - cc]])
        cfin = NQ % 2
        bi = prot.next()
        mm(pb[bi][:, 0:32], pones, Cacc[cfin], True, True, [b_cb, b_Cacc[cfin]], [b_pb[bi]])
        ntot = AR.alloc([128, 32], F32)
        nti = AR.alloc([128, 32], I32)
        npad = AR.alloc([128, 32], F32)
        offs = AR.alloc([128, 33], F32)
        b_off = Buf()
        ts("dve", ntot, pb[bi][:, 0:32], 255.0, None, ALU.add, None, [b_pb[bi]], [b_off])
        cp("dve", nti, ntot, [b_off], [b_off])
        P.dve(lambda e, o=nti: e.tensor_single_scalar(out=o, in_=o, scalar=8, op=ALU.arith_shift_right), [b_off], [b_off])
        P.dve(lambda e, o=nti: e.tensor_single_scalar(out=o, in_=o, scalar=8, op=ALU.logical_shift_left), [b_off], [b_off])
        cp("dve", npad, nti, [b_off], [b_off])
        P.dve(lambda e, o=offs[:, 0:1]: e.memset(o, 0.0), [], [b_off])
        for e_i in range(32):
            tt("dve", offs[:, e_i + 1:e_i + 2], offs[:, e_i:e_i + 1], npad[:, e_i:e_i + 1], ALU.add, [b_off], [b_off])
        posf = AR.alloc([128, NQ], F32)
        posi = AR.alloc([128, NQ], I32)
        b_pos = Buf()
        for q in range(NQ):
            cc = q % 2
            tt("dve", tmp32[cc], offs[:, 0:32], OHall[:, q, :], ALU.mult, [b_off, b_OH[q]], [b_tmp32[cc]])
            P.dve(lambda e, o=posf[:, q:q + 1], i_=tmp32[cc]: e.tensor_reduce(out=o, in_=i_, axis=AX.X, op=ALU.add), [b_tmp32[cc]], [b_pos])
        tt("dve", posf, posf, rk, ALU.add, [b_pos, b_rk], [b_pos])
        cp("dve", posi, posf, [b_pos], [b_pos])
        hi_f = AR.alloc([128, NQ], F32)
        P.dve(lambda e, o=posi: e.tensor_single_scalar(out=o, in_=o, scalar=7, op=ALU.arith_shift_right), [b_pos], [b_pos])
        cp("dve", hi_f, posi, [b_pos], [b_pos])
        stt("dve", posf, hi_f, -128.0, posf, ALU.mult, ALU.add, [b_pos], [b_pos])
        stt("dve", posf, posf, float(NSLOT), hi_f, ALU.mult, ALU.add, [b_pos], [b_pos])
        cp("dve", posi, posf, [b_pos], [b_pos])
        pidt = AR.alloc([128, NQ], I32)
        b_pidt = Buf()
        dma(pidt, pairid_d, [], [b_pidt])
        linit = AR.alloc([128, NSLOT], I32)
        b_linit = Buf()
        dma(linit, linit_d, [], [b_linit])
        dma(lst_i.rearrange("(p s) c -> p (s c)", p=128), linit, [b_linit], [b_lst])
        for q in range(NQ):
            P.dma(lambda e, q=q: e.indirect_dma_start(out=lst_i[:, :], out_offset=bass.IndirectOffsetOnAxis(ap=posi[:, q:q + 1], axis=0),
                  in_=pidt[:, q:q + 1], in_offset=None, bounds_check=None),
                  [b_pos, b_pidt, b_lst], [], q="pool")
            P.dma(lambda e, q=q: e.indirect_dma_start(out=lst_g[:, :], out_offset=bass.IndirectOffsetOnAxis(ap=posi[:, q:q + 1], axis=0),
                  in_=gall[:, q:q + 1], in_offset=None, bounds_check=None),
                  [b_pos, b_gall[q], b_lst], [], q="pool")
        lsb_i = AR.alloc([128, NSLOT], I32)
        lsb_g = AR.alloc([128, NSLOT], F32)
        b_lsb = Buf()
        dma(lsb_i, lst_i.rearrange("(p s) c -> p (s c)", p=128), [], [b_lsb, b_lst])
        dma(lsb_g, lst_g.rearrange("(p s) c -> p (s c)", p=128), [], [b_lsb, b_lst])
        sst = AR.alloc([128, NSLOT], F32)
        sef = AR.alloc([128, NSLOT], F32)
        widx = AR.alloc([128, NSLOT], I32)
        piota = AR.alloc([128, 1], F32)
        b_se = Buf()
        dma(sst, sstart_d, [], [b_se])
        dma(piota, piota_d, [], [b_se])
        P.dve(lambda e, o=sef: e.memset(o, 0.0), [], [b_se])
        for e_i in range(32):
            stt("dve", sef, sst, offs[:, e_i + 1:e_i + 2], sef, ALU.is_ge, ALU.add, [b_se, b_off], [b_se])
        ts("dve", sef, sef, 31.0, None, ALU.min, None, [b_se], [b_se])
        ts("dve", sef, sef, 128.0, None, ALU.mult, None, [b_se], [b_se])
        ts("dve", sef, sef, piota, None, ALU.add, None, [b_se], [b_se])
        cp("dve", widx, sef, [b_se], [b_se])
        sort_mark = AR.off
        if stop_after == "sort":
            P.emit(nc, st)
            return nc
        NBUF = 3
        Wg = [AR.alloc([128, 4096], BF16) for _ in range(NBUF)]
        Wu = [AR.alloc([128, 4096], BF16) for _ in range(NBUF)]
        Wd = [AR.alloc([128, 4096], BF16) for _ in range(NBUF)]
        b_W = [Buf() for _ in range(NBUF)]
        xg = [AR.alloc([128, D], BF16) for _ in range(2 * NBUF)]
        b_xg = [Buf() for _ in range(2 * NBUF)]
        xgT = [AR.alloc([128, 8, 128], BF16) for _ in range(2)]
        b_xgT = [Buf() for _ in range(2)]
        sgs = [AR.alloc([128, 512], F32) for _ in range(2)]
        b_sgs = [Buf() for _ in range(2)]
        hs_ = [AR.alloc([128, 512], BF16) for _ in range(2)]
        b_hs = [Buf() for _ in range(2)]
        hTs = [AR.alloc([128, 4, 128], BF16) for _ in range(2)]
        b_hTs = [Buf() for _ in range(2)]
        ysb = [AR.alloc([128, D], F32) for _ in range(2)]
        b_ysb = [Buf() for _ in range(2)]
        for xi_, xg_ in enumerate(xg):
            P.dve(lambda e, o=xg_: e.memset(o, 0.0), [], [b_xg[xi_]])

        def igather(dst, src, idx_ap, bound, r, w):
            P.dma(lambda e, dst=dst, src=src, idx_ap=idx_ap, bound=bound: e.indirect_dma_start(
                out=dst, out_offset=None, in_=src, in_offset=bass.IndirectOffsetOnAxis(ap=idx_ap, axis=0),
                bounds_check=None), r, w, q="pool")

        def slot_gathers(J):
            s_ = J % NBUF
            for h_ in range(2):
                j = 2 * J + h_
                xi = 2 * s_ + h_
                igather(xg[xi][:, :], m_scr[:, :], lsb_i[:, j:j + 1], 2 * S - 1, [b_lsb, b_xg[xi]], [b_xg[xi]])
            igather(Wg[s_][:, :], wgb[:, :], widx[:, 2 * J:2 * J + 1], 32 * 128 - 1, [b_se], [b_W[s_]])
            igather(Wu[s_][:, :], wub[:, :], widx[:, 2 * J:2 * J + 1], 32 * 128 - 1, [b_se], [b_W[s_]])
            igather(Wd[s_][:, :], wdb[:, :], widx[:, 2 * J:2 * J + 1], 32 * 128 - 1, [b_se], [b_W[s_]])

        def slot_compute(j):
            s_ = (j // 2) % NBUF
            d_ = j % 2
            xi = 2 * s_ + d_
            bi = prot.next()
            pv = bf16view(pb[bi]).rearrange("p (k n) -> p k n", k=8)
            for k in range(8):
                tr(pv[:, k, :], xg[xi][:, k * 128:(k + 1) * 128], idb, [b_xg[xi], b_cb], [b_pb[bi]])
            cp("act", xgT[d_], pv, [b_pb[bi]], [b_xgT[d_]])
            bg = prot.next()
            for k in range(8):
                mm(pb[bg][:, :], xgT[d_][:, k, :], Wg[s_][:, k * 512:(k + 1) * 512], k == 0, k == 7, [b_xgT[d_], b_W[s_]], [b_pb[bg]])
            bu = prot.next()
            for k in range(8):
                mm(pb[bu][:, :], xgT[d_][:, k, :], Wu[s_][:, k * 512:(k + 1) * 512], k == 0, k == 7, [b_xgT[d_], b_W[s_]], [b_pb[bu]])
            actf(sgs[d_], pb[bg][:, :], AF.Silu, [b_pb[bg]], [b_sgs[d_]])
            tt("dve", hs_[d_], sgs[d_], pb[bu][:, :], ALU.mult, [b_sgs[d_], b_pb[bu]], [b_hs[d_]])
            bi = prot.next()
            ph = bf16view(pb[bi])[:, 0:512].rearrange("p (k n) -> p k n", k=4)
            for k in range(4):
                tr(ph[:, k, :], hs_[d_][:, k * 128:(k + 1) * 128], idb, [b_hs[d_], b_cb], [b_pb[bi]])
            cp("act", hTs[d_], ph, [b_pb[bi]], [b_hTs[d_]])
            gate_ap = lsb_g[:, j:j + 1]
            for half in range(2):
                by = prot.next()
                for fc in range(4):
                    mm(pb[by][:, :], hTs[d_][:, fc, :], Wd[s_][:, fc * 1024 + half * 512:fc * 1024 + (half + 1) * 512],
                       fc == 0, fc == 3, [b_hTs[d_], b_W[s_]], [b_pb[by]])
                ts("dve", ysb[d_][:, half * 512:(half + 1) * 512], pb[by][:, :], gate_ap, None, ALU.mult, None,
                   [b_pb[by], b_lsb], [b_ysb[d_]])
            P.dma(lambda e, j=j, d_=d_: e.indirect_dma_start(out=y_scr[:, :], out_offset=bass.IndirectOffsetOnAxis(ap=lsb_i[:, j:j + 1], axis=0),
                  in_=ysb[d_][:, :], in_offset=None, bounds_check=None),
                  [b_lsb, b_ysb[d_]], [], q="pool")

        NBIG = NSLOT // 2
        slot_gathers(0)
        slot_gathers(1)
        for J in range(NBIG):
            if J + 2 < NBIG:
                slot_gathers(J + 2)
            slot_compute(2 * J)
            slot_compute(2 * J + 1)
        if stop_after == "slots":
            P.emit(nc, st)
            return nc
        P.barrier()
        AR.off = pass_mark
        wpg = AR.alloc([128, 8, D], BF16)
        wpp = AR.alloc([128, 2, D], BF16)
        b_wp = Buf()
        dma(wpg, w_pg.rearrange("(k p) n -> p k n", p=128), [], [b_wp], q="pool")
        dma(wpp, w_pp.rearrange("(k p) n -> p k n", p=128), [], [b_wp], q="pool")
        pts = [AR.alloc([128, 256], F32) for _ in range(WB)]
        b_pts = [Buf() for _ in range(WB)]
        ptb = [AR.alloc([128, 256], BF16) for _ in range(WB)]
        b_ptb = [Buf() for _ in range(WB)]
        ppT = [AR.alloc([128, 2, 128], BF16) for _ in range(WB)]
        b_ppT = [Buf() for _ in range(WB)]
        h2s = [AR.alloc([128, D], F32) for _ in range(WB)]
        b_h2s = [Buf() for _ in range(WB)]
        y0s = [AR.alloc([128, D], F32) for _ in range(WB)]
        y1s = [AR.alloc([128, D], F32) for _ in range(WB)]
        b_ys = [Buf() for _ in range(WB)]
        h2b = [AR.alloc([128, D], BF16) for _ in range(WB)]
        b_h2b = [Buf() for _ in range(WB)]
        h2T = [AR.alloc([128, 8, 128], BF16) for _ in range(WB)]
        b_h2T = [Buf() for _ in range(WB)]
        gts = [AR.alloc([128, D], F32) for _ in range(WB)]
        b_gts = [Buf() for _ in range(WB)]
        h3s = [AR.alloc([128, D], F32) for _ in range(WB)]
        b_h3s = [Buf() for _ in range(WB)]
        ots = [AR.alloc([128, D], F32) for _ in range(WB)]
        b_ots = [Buf() for _ in range(WB)]
        r3 = [AR.alloc([128, 4], F32) for _ in range(WB)]
        b_r3 = [Buf() for _ in range(WB)]
        def b3_tile(t):
            s_ = t % WB
            h2 = h2s[s_]
            bh2 = b_h2s[s_]
            dma(h2, h1_scr[t * 128:(t + 1) * 128, :], [], [bh2])
            dma(y0s[s_], y_scr[t * 128:(t + 1) * 128, :], [], [b_ys[s_]], q="act")
            dma(y1s[s_], y_scr[S + t * 128:S + (t + 1) * 128, :], [], [b_ys[s_]], q="act")
            dma(pts[s_], p_in[t * 128:(t + 1) * 128, :], [], [b_pts[s_]])
            tt("pool", y0s[s_], y0s[s_], y1s[s_], ALU.add, [b_ys[s_]], [b_ys[s_]])
            tt("dve", h2, h2, y0s[s_], ALU.add, [bh2, b_ys[s_]], [bh2])
            yield
            ss2 = r3[s_][:, 0:1]
            nrs2 = r3[s_][:, 1:2]
            ss3 = r3[s_][:, 2:3]
            R3 = [b_r3[s_]]
            actf(junkB, h2, AF.Square, [bh2], [b_junkB] + R3, accum=ss2)
            rstd_from_ss(ss2, D, b_r3[s_])
            yield
            ts("dve", nrs2, ss2, -1.0, None, ALU.mult, None, R3, R3)
            cp("act", h2b[s_], h2, [bh2], [b_h2b[s_]])
            yield
            bi = prot.next()
            pv = bf16view(pb[bi]).rearrange("p (k n) -> p k n", k=8)
            for k in range(8):
                tr(pv[:, k, :], h2b[s_][:, k * 128:(k + 1) * 128], idb, [b_h2b[s_], b_cb], [b_pb[bi]])
            tt("dve", h2T[s_], pv, plwT.unsqueeze(2).to_broadcast([128, 8, 128]), ALU.mult, [b_pb[bi], b_nw2], [b_h2T[s_]])
            yield
            cp("pool", ptb[s_], pts[s_], [b_pts[s_]], [b_ptb[s_]])
            yield
            bi = prot.next()
            pv2 = bf16view(pb[bi]).rearrange("p (k n) -> p k n", k=8)
            for k in range(2):
                tr(pv2[:, k, :], ptb[s_][:, k * 128:(k + 1) * 128], idb, [b_ptb[s_], b_cb], [b_pb[bi]])
            cp("act", ppT[s_], pv2[:, 0:2, :], [b_pb[bi]], [b_ppT[s_]])
            yield
            for half in range(2):
                hsl = slice(half * 512, (half + 1) * 512)
                bg = prot.next()
                for k in range(8):
                    mm(pb[bg][:, :], h2T[s_][:, k, :], wpg[:, k, hsl], k == 0, k == 7, [b_h2T[s_], b_wp], [b_pb[bg]])
                be = prot.next()
                for k in range(2):
                    mm(pb[be][:, :], ppT[s_][:, k, :], wpp[:, k, hsl], k == 0, k == 1, [b_ppT[s_], b_wp], [b_pb[be]])
                gt_ = gts[s_][:, hsl]
                actf(gt_, pb[bg][:, :], AF.Exp, [b_pb[bg]] + R3, [b_gts[s_]], scale=nrs2)
                ts("dve", gt_, gt_, 1.0, None, ALU.add, None, [b_gts[s_]], [b_gts[s_]])
                recip_act(gt_, gt_, [b_gts[s_]], [b_gts[s_]])
                tt("dve", gt_, gt_, pb[be][:, :], ALU.mult, [b_gts[s_], b_pb[be]], [b_gts[s_]])
                tt("pool", h3s[s_][:, hsl], gt_, h2[:, hsl], ALU.add, [b_gts[s_], bh2], [b_h3s[s_]])
                yield
            actf(junkB, h3s[s_], AF.Square, [b_h3s[s_]], [b_junkB] + R3, accum=ss3)
            rstd_from_ss(ss3, D, b_r3[s_])
            yield
            stt("dve", ots[s_], h3s[s_], ss3, finw_b, ALU.mult, ALU.mult, [b_h3s[s_], b_finw] + R3, [b_ots[s_]])
            dma(out[t * 128:(t + 1) * 128, :], ots[s_], [b_ots[s_]], [])
        run_interleaved([b3_tile(t) for t in range(NT)], WB)
        P.emit(nc, st)
    return nc


def _consts():
    cb = np.zeros((128, NCB), np.float32)
    j = np.arange(128)[:, None]
    s = np.arange(128)[None, :]
    cb[:, 0:128] = np.eye(128, dtype=np.float32)
    cb[:, 128:256] = np.where(j >= s, -1.0, 0.0)
    cb[:, 256:384] = -1.0
    c = np.arange(512)[None, :]
    for jj in range(4):
        cb[:, 640 + 512 * jj:640 + 512 * (jj + 1)] = np.where(c > j + 128 * jj, 0.0, -30000.0)
    cb[:64, 2688] = 1.0
    cb[64:, 2689] = 1.0
    cc = np.arange(512)
    cb[:, 2696:2696 + 512] = ((cc % 128) < 64).astype(np.float32)[None, :]
    cb[:, 2696 + 512:2696 + 1024] = ((cc % 128) >= 64).astype(np.float32)[None, :]
    cb[:, 3720:3848] = 1.0
    cf = np.zeros((128, NCF), np.float32)
    same = (j // 64) == (s // 64)
    cb[:, 384:512] = np.where((j <= s) & same, 1.0, 0.0)
    cb[:, 512:640] = np.where((j > s) & same, 1.0, 0.0)
    return cb, cf


def make_in_maps(inputs, S, n):
    f = lambda a: np.ascontiguousarray(np.asarray(a, dtype=np.float32))
    cb, cf = _consts()
    shared = {
        "w_in": f(inputs["w_in"][0]), "w_out": f(inputs["w_out"][0]),
        "w_r": f(np.concatenate([np.asarray(inputs["w_group_router"][0]), np.asarray(inputs["w_expert_router"][0])], axis=1)),
        "b_r": f(np.concatenate([np.asarray(inputs["b_group_router"][0]), np.asarray(inputs["b_expert_router"][0])], axis=0)),
        "w_eg": f(inputs["w_exp_gate"][0]), "w_eu": f(inputs["w_exp_up"][0]), "w_ed": f(inputs["w_exp_down"][0]),
        "w_pp": f(inputs["w_ple_proj"][0]), "w_pg": f(inputs["w_ple_gate"][0]),
        "anw": f(np.asarray(inputs["attn_norm_w"][0]).reshape(8, 128).T), "sbw": f(inputs["sb_norm_w"][0]), "hlb": f(inputs["hg_lower_bounds"]),
        "hgw": f(inputs["hg_norm_w"][0]), "fnw": f(np.asarray(inputs["ffn_norm_w"][0]).reshape(8, 128).T), "plw": f(np.asarray(inputs["ple_norm_w"][0]).reshape(8, 128).T),
        "finw": f(inputs["final_norm_w"]), "fnwv": f(inputs["ffn_norm_w"][0]), "cb": cb, "cf": cf,
    }
    NT_ = S // 128
    NQ_ = 2 * NT_
    NSL_ = NQ_ + 64
    shared["pairid"] = (np.arange(NQ_, dtype=np.int32)[None, :] * 128 + np.arange(128, dtype=np.int32)[:, None]).astype(np.int32)
    li = np.broadcast_to((2 * S + np.arange(128, dtype=np.int32))[:, None], (128, NSL_)).astype(np.int32).copy()
    shared["linit"] = li
    shared["sstart"] = np.broadcast_to((np.arange(NSL_, dtype=np.float32) * 128.0)[None, :], (128, NSL_)).copy()
    shared["piota"] = np.arange(128, dtype=np.float32).reshape(128, 1)
    maps = []
    for b in range(n):
        m = dict(shared)
        m["x"] = f(np.asarray(inputs["x"])[b, :S])
        m["p"] = f(np.asarray(inputs["p"])[0, b, :S])
        maps.append(m)
    return maps


def kernel(**inputs):
    S = 4096
    nc = build(S)
    maps = make_in_maps(inputs, S, 8)
    res = run_bass_kernel_spmd(nc, maps, core_ids=list(range(8)))
    return np.stack([np.asarray(r["out"], dtype=np.float32) for r in res.results], axis=0)
```
